# Optimizing a Trainium2 kernel written in Bass

```python
import math
import jax, jax.numpy as jnp
from jax import lax
import numpy as np

D_MODEL = 1024
BATCH = 8
SEQ = 4096
DEPTH = 4

GLA_HEADS = 4
GLA_DK = 32
GLA_DV = 64
GLA_GATE_RANK = 16
GLA_TAU = 16.0
GLA_CHUNK = 64
DIFF_HEADS = 4
DIFF_D = 64
DIFF_QBLOCK = 128
RET_HEADS = 4
RET_DK = 64
RET_DV = 64
RET_CHUNK = 128
T5_BUCKETS = 32
T5_MAX_DIST = 128
N_EXPERTS = 32
TOP_K = 4
D_FF = D_MODEL
SWIGLU_LIMIT = 7.0
SWIGLU_ALPHA = 1.702
MOE_BLOCK = 256
PLE_DIM = 256
DEEPNORM_ALPHA = (2 * DEPTH) ** 0.25
DEEPNORM_BETA = (8 * DEPTH) ** -0.25
LN_EPS = 1e-5
HEAD_NORM_EPS = 1e-5

MIX_WIDTH = GLA_HEADS * GLA_DV + DIFF_HEADS * 2 * DIFF_D + RET_HEADS * RET_DV
IN_SPLITS = (GLA_HEADS * GLA_DK, GLA_HEADS * GLA_DK, GLA_HEADS * GLA_DV, GLA_GATE_RANK, GLA_HEADS * GLA_DV,
             DIFF_HEADS * 2 * DIFF_D, DIFF_HEADS * 2 * DIFF_D, DIFF_HEADS * 2 * DIFF_D,
             RET_HEADS * RET_DK, RET_HEADS * RET_DK, RET_HEADS * RET_DV, RET_HEADS * RET_DV)
IN_WIDTH = sum(IN_SPLITS)
IN_OFFSETS = tuple(int(o) for o in np.cumsum(IN_SPLITS)[:-1])

kernel_name = "hymba_gla_diff_retnet_moe_deepnorm"


def layer_norm(x, g, b):
    xf = x.astype(jnp.float32)
    mu = jnp.mean(xf, axis=-1, keepdims=True)
    var = jnp.mean(jnp.square(xf - mu), axis=-1, keepdims=True)
    y = (xf - mu) * lax.rsqrt(var + LN_EPS) * g.astype(jnp.float32) + b.astype(jnp.float32)
    return y.astype(x.dtype)


def head_rms_norm(x, g=None):
    xf = x.astype(jnp.float32)
    y = xf * lax.rsqrt(jnp.mean(jnp.square(xf), axis=-1, keepdims=True) + HEAD_NORM_EPS)
    if g is not None:
        y = y * g.astype(jnp.float32)
    return y


def to_chunks(t, c):
    b, s, h, d = t.shape
    return t.reshape(b, s // c, c, h, d).transpose(1, 0, 3, 2, 4)


def from_chunks(t):
    n, b, h, c, d = t.shape
    return t.transpose(1, 0, 3, 2, 4).reshape(b, n * c, h, d)


def t5_bucket(rel):
    n = jnp.maximum(-rel, 0)
    max_exact = T5_BUCKETS // 2
    nf = jnp.maximum(n, 1).astype(jnp.float32)
    large = max_exact + (jnp.log(nf / max_exact) / math.log(T5_MAX_DIST / max_exact)
                         * (T5_BUCKETS - max_exact)).astype(jnp.int32)
    large = jnp.minimum(large, T5_BUCKETS - 1)
    return jnp.where(n < max_exact, n, large)


def gla_group(q, k, v, gate_lr, out_gate, w_gate, b_gate, norm_g):
    b_, s_ = q.shape[:2]
    log_a = jax.nn.log_sigmoid((gate_lr @ w_gate + b_gate).astype(jnp.float32)) / GLA_TAU
    log_a = log_a.reshape(b_, s_, GLA_HEADS, GLA_DK)
    qc = to_chunks(q.astype(jnp.float32) * GLA_DK ** -0.5, GLA_CHUNK)
    kc = to_chunks(k.astype(jnp.float32), GLA_CHUNK)
    vc = to_chunks(v.astype(jnp.float32), GLA_CHUNK)
    lc = to_chunks(log_a, GLA_CHUNK)
    causal = jnp.tril(jnp.ones((GLA_CHUNK, GLA_CHUNK), dtype=bool))

    def step(state, inp):
        q_c, k_c, v_c, la_c = inp
        cum = jnp.cumsum(la_c, axis=2)
        diff = cum[:, :, :, None, :] - cum[:, :, None, :, :]
        decay = jnp.exp(jnp.where(causal[:, :, None], diff, -jnp.inf))
        scores = jnp.einsum('bhtc,bhsc,bhtsc->bhts', q_c, k_c, decay)
        o = scores @ v_c + jnp.einsum('bhtc,bhcv->bhtv', q_c * jnp.exp(cum), state)
        last = cum[:, :, -1:, :]
        state = jnp.exp(last[:, :, 0, :])[..., None] * state + \
            jnp.einsum('bhsc,bhsv->bhcv', k_c * jnp.exp(last - cum), v_c)
        return state, o

    s0 = jnp.zeros((b_, GLA_HEADS, GLA_DK, GLA_DV), jnp.float32)
    _, o = lax.scan(step, s0, (qc, kc, vc, lc))
    o = head_rms_norm(from_chunks(o), norm_g).reshape(b_, s_, GLA_HEADS * GLA_DV)
    return o * jax.nn.silu(out_gate.astype(jnp.float32))


def diff_group(q, k, v, lam, lam_init, norm_g, rel_bias):
    b_, s_ = q.shape[:2]
    nq = s_ // DIFF_QBLOCK
    qh = q.astype(jnp.float32).transpose(0, 2, 3, 1, 4) * DIFF_D ** -0.5
    kh = k.astype(jnp.float32).transpose(0, 2, 3, 1, 4)
    vh = v.astype(jnp.float32).transpose(0, 2, 1, 3)
    qb = qh.reshape(b_, DIFF_HEADS, 2, nq, DIFF_QBLOCK, DIFF_D).transpose(3, 0, 1, 2, 4, 5)
    kpos = jnp.arange(s_)

    def block(args):
        q_blk, blk = args
        qpos = blk * DIFF_QBLOCK + jnp.arange(DIFF_QBLOCK)
        rel = kpos[None, :] - qpos[:, None]
        bias = rel_bias[t5_bucket(rel)].astype(jnp.float32).transpose(2, 0, 1)
        logits = jnp.einsum('bhmqd,bhmkd->bhmqk', q_blk, kh) + bias[None, :, None]
        logits = jnp.where((rel <= 0)[None, None, None], logits, -jnp.inf)
        probs = jax.nn.softmax(logits, axis=-1)
        attn = probs[:, :, 0] - lam * probs[:, :, 1]
        return jnp.einsum('bhqk,bhke->bhqe', attn, vh)

    o = lax.map(block, (qb, jnp.arange(nq)))
    o = o.transpose(1, 0, 3, 2, 4).reshape(b_, s_, DIFF_HEADS, 2 * DIFF_D)
    o = head_rms_norm(o, norm_g) * (1.0 - lam_init)
    return o.reshape(b_, s_, DIFF_HEADS * 2 * DIFF_D)


def rotate_every_two(t):
    t1 = t[..., ::2]
    t2 = t[..., 1::2]
    return jnp.stack((-t2, t1), axis=-1).reshape(t.shape)


def retention_group(q, k, v, gate):
    b_, s_ = q.shape[:2]
    pos = jnp.arange(s_, dtype=jnp.float32)
    angle = 1.0 / (10000.0 ** jnp.linspace(0.0, 1.0, RET_DK // 2, dtype=jnp.float32))
    angle = jnp.repeat(angle, 2)
    sin = jnp.sin(pos[:, None] * angle)[None, :, None, :]
    cos = jnp.cos(pos[:, None] * angle)[None, :, None, :]
    qf = q.astype(jnp.float32)
    kf = k.astype(jnp.float32)
    qf = qf * cos + rotate_every_two(qf) * sin
    kf = (kf * cos + rotate_every_two(kf) * sin) * RET_DK ** -0.5
    log_g = jnp.log1p(-jnp.exp2(-5.0 - jnp.arange(RET_HEADS, dtype=jnp.float32)))
    idx = jnp.arange(RET_CHUNK, dtype=jnp.float32)
    rel = idx[:, None] - idx[None, :]
    inner_decay = jnp.where(rel[None] >= 0, jnp.exp(jnp.maximum(rel, 0.0)[None] * log_g[:, None, None]), 0.0)
    cross_decay = jnp.exp((idx + 1.0)[None] * log_g[:, None])
    state_decay = jnp.exp((RET_CHUNK - 1.0 - idx)[None] * log_g[:, None])
    chunk_decay = jnp.exp(RET_CHUNK * log_g)

    def step(state, inp):
        q_c, k_c, v_c = inp
        scores = jnp.einsum('bhtd,bhsd->bhts', q_c, k_c) * inner_decay
        o = scores @ v_c + jnp.einsum('bhtd,bhdv->bhtv', q_c, state) * cross_decay[:, :, None]
        state = state * chunk_decay[:, None, None] + \
            jnp.einsum('bhsd,bhsv->bhdv', k_c * state_decay[:, :, None], v_c)
        return state, o

    s0 = jnp.zeros((b_, RET_HEADS, RET_DK, RET_DV), jnp.float32)
    _, o = lax.scan(step, s0, (to_chunks(qf, RET_CHUNK), to_chunks(kf, RET_CHUNK),
                               to_chunks(v.astype(jnp.float32), RET_CHUNK)))
    o = head_rms_norm(from_chunks(o)).reshape(b_, s_, RET_HEADS * RET_DV)
    return o * jax.nn.silu(gate.astype(jnp.float32))


def hybrid_mixer(x, w_in, w_gla_gate, b_gla_gate, gla_norm_g, diff_lambda, diff_norm_g,
                 w_out, rel_bias, lam_init):
    b_, s_, _ = x.shape
    z = x @ w_in
    (gq, gk, gv, g_lr, g_out, dq, dk, dv, rq, rk, rv, rg) = jnp.split(z, IN_OFFSETS, axis=-1)
    gla_o = gla_group(gq.reshape(b_, s_, GLA_HEADS, GLA_DK), gk.reshape(b_, s_, GLA_HEADS, GLA_DK),
                      gv.reshape(b_, s_, GLA_HEADS, GLA_DV), g_lr, g_out,
                      w_gla_gate, b_gla_gate, gla_norm_g)
    lf = diff_lambda.astype(jnp.float32)
    lam = jnp.exp(jnp.sum(lf[0] * lf[1])) - jnp.exp(jnp.sum(lf[2] * lf[3])) + lam_init
    diff_o = diff_group(dq.reshape(b_, s_, DIFF_HEADS, 2, DIFF_D), dk.reshape(b_, s_, DIFF_HEADS, 2, DIFF_D),
                        dv.reshape(b_, s_, DIFF_HEADS, 2 * DIFF_D), lam, lam_init, diff_norm_g, rel_bias)
    ret_o = retention_group(rq.reshape(b_, s_, RET_HEADS, RET_DK), rk.reshape(b_, s_, RET_HEADS, RET_DK),
                            rv.reshape(b_, s_, RET_HEADS, RET_DV), rg)
    mixed = jnp.concatenate([gla_o, diff_o, ret_o], axis=-1).astype(x.dtype)
    return mixed @ w_out


def moe_ffn(h, w_router, b_router, w_gate_up, b_gate_up, w_down, b_down):
    n_tok, d = h.shape
    logits = (h @ w_router + b_router).astype(jnp.float32)
    top_v, top_i = lax.top_k(logits, TOP_K)
    gates = jax.nn.softmax(top_v, axis=-1)
    n_assign = n_tok * TOP_K
    e_flat = top_i.reshape(-1)
    tok_flat = jnp.repeat(jnp.arange(n_tok, dtype=jnp.int32), TOP_K)
    g_flat = gates.reshape(-1)
    order = jnp.argsort(e_flat)
    se, stok, sg = e_flat[order], tok_flat[order], g_flat[order]
    sizes = jnp.bincount(e_flat, length=N_EXPERTS)
    starts = jnp.cumsum(sizes) - sizes
    padded = ((sizes + MOE_BLOCK - 1) // MOE_BLOCK) * MOE_BLOCK
    pends = jnp.cumsum(padded)
    pstarts = pends - padded
    dest = pstarts[se] + (jnp.arange(n_assign) - starts[se])
    buf_len = ((n_assign + N_EXPERTS * (MOE_BLOCK - 1) + MOE_BLOCK - 1) // MOE_BLOCK) * MOE_BLOCK
    n_blk = buf_len // MOE_BLOCK
    buf_tok = jnp.full((buf_len,), n_tok, jnp.int32).at[dest].set(stok)
    buf_g = jnp.zeros((buf_len,), jnp.float32).at[dest].set(sg)
    blk_e = jnp.clip(jnp.searchsorted(pends, jnp.arange(n_blk) * MOE_BLOCK, side='right'), 0, N_EXPERTS - 1)
    h_pad = jnp.concatenate([h, jnp.zeros((1, d), h.dtype)], axis=0)

    def expert_block(args):
        tok, e = args
        hb = h_pad[tok]
        gu = hb @ w_gate_up[e] + b_gate_up[e]
        g = jnp.minimum(gu[:, ::2], SWIGLU_LIMIT)
        u = jnp.clip(gu[:, 1::2], -SWIGLU_LIMIT, SWIGLU_LIMIT)
        act = (u + 1.0) * (g * jax.nn.sigmoid(g * SWIGLU_ALPHA))
        return act @ w_down[e] + b_down[e]

    yb = lax.map(expert_block, (buf_tok.reshape(n_blk, MOE_BLOCK), blk_e))
    contrib = yb.reshape(buf_len, d).astype(jnp.float32) * buf_g[:, None]
    y = jax.ops.segment_sum(contrib, buf_tok, num_segments=n_tok + 1)[:n_tok]
    return y.astype(h.dtype)


def setup_inputs(seed: int = 0) -> dict:
    key = jax.random.key(seed)
    ks = jax.random.split(key, 24)
    f32 = jnp.float32

    def nrm(k, shape, scale):
        return jax.random.normal(k, shape, f32) * scale

    return {
        "x": nrm(ks[0], (BATCH, SEQ, D_MODEL), 1.0),
        "p": nrm(ks[1], (DEPTH, BATCH, SEQ, PLE_DIM), 1.0),
        "w_in": nrm(ks[2], (DEPTH, D_MODEL, IN_WIDTH), D_MODEL ** -0.5),
        "w_gla_gate": nrm(ks[3], (DEPTH, GLA_GATE_RANK, GLA_HEADS * GLA_DK), GLA_GATE_RANK ** -0.5),
        "b_gla_gate": nrm(ks[4], (DEPTH, GLA_HEADS * GLA_DK), 0.1),
        "gla_norm_g": 1.0 + nrm(ks[5], (DEPTH, GLA_DV), 0.02),
        "diff_lambda": nrm(ks[6], (DEPTH, 4, DIFF_D), 0.1),
        "diff_norm_g": 1.0 + nrm(ks[7], (DEPTH, 2 * DIFF_D), 0.02),
        "w_out": nrm(ks[8], (DEPTH, MIX_WIDTH, D_MODEL), MIX_WIDTH ** -0.5 * DEEPNORM_BETA),
        "rel_bias": nrm(ks[9], (T5_BUCKETS, DIFF_HEADS), 0.5),
        "ln1_g": 1.0 + nrm(ks[10], (DEPTH, D_MODEL), 0.02),
        "ln1_b": nrm(ks[11], (DEPTH, D_MODEL), 0.02),
        "w_router": nrm(ks[12], (DEPTH, D_MODEL, N_EXPERTS), D_MODEL ** -0.5),
        "b_router": nrm(ks[13], (DEPTH, N_EXPERTS), 0.01),
        "w_gate_up": nrm(ks[14], (DEPTH, N_EXPERTS, D_MODEL, 2 * D_FF), D_MODEL ** -0.5),
        "b_gate_up": nrm(ks[15], (DEPTH, N_EXPERTS, 2 * D_FF), 0.01),
        "w_down": nrm(ks[16], (DEPTH, N_EXPERTS, D_FF, D_MODEL), D_FF ** -0.5 * DEEPNORM_BETA),
        "b_down": nrm(ks[17], (DEPTH, N_EXPERTS, D_MODEL), 0.01),
        "w_ple_gate": nrm(ks[18], (DEPTH, D_MODEL, D_MODEL), D_MODEL ** -0.5),
        "b_ple_gate": nrm(ks[19], (DEPTH, D_MODEL), 0.01),
        "w_ple_proj": nrm(ks[20], (DEPTH, PLE_DIM, D_MODEL), PLE_DIM ** -0.5 * DEEPNORM_BETA),
        "ln2_g": 1.0 + nrm(ks[21], (DEPTH, D_MODEL), 0.02),
        "ln2_b": nrm(ks[22], (DEPTH, D_MODEL), 0.02),
    }


def reference(x, p, w_in, w_gla_gate, b_gla_gate, gla_norm_g, diff_lambda, diff_norm_g, w_out,
              rel_bias, ln1_g, ln1_b, w_router, b_router, w_gate_up, b_gate_up, w_down, b_down,
              w_ple_gate, b_ple_gate, w_ple_proj, ln2_g, ln2_b):
    b_, s_, d = x.shape
    for i in range(DEPTH):
        lam_init = 0.8 - 0.6 * math.exp(-0.3 * i)
        a = hybrid_mixer(x, w_in[i], w_gla_gate[i], b_gla_gate[i], gla_norm_g[i], diff_lambda[i],
                         diff_norm_g[i], w_out[i], rel_bias, lam_init)
        x = layer_norm(DEEPNORM_ALPHA * x + a, ln1_g[i], ln1_b[i])
        m = moe_ffn(x.reshape(b_ * s_, d), w_router[i], b_router[i], w_gate_up[i], b_gate_up[i],
                    w_down[i], b_down[i]).reshape(b_, s_, d)
        e = jax.nn.sigmoid(x @ w_ple_gate[i] + b_ple_gate[i]) * (p[i] @ w_ple_proj[i])
        x = layer_norm(DEEPNORM_ALPHA * x + m + e, ln2_g[i], ln2_b[i])
    return x
```

```python
import math
import types
import numpy as np
import concourse.bass as bass
import concourse.mybir as mybir
from concourse.bass_utils import run_bass_kernel_spmd

F32 = mybir.dt.float32
BF16 = mybir.dt.bfloat16
I32 = mybir.dt.int32
ALU = mybir.AluOpType
AF = mybir.ActivationFunctionType
AX = mybir.AxisListType

D = 1024
NE = 32
PLE = 256
ALPHA = 8 ** 0.25
LN_EPS = 1e-5
HN_EPS = 1e-5
SEM_LIMIT = 12000
N_DMA_SEMS = 12
NEG = -30000.0
BIGPOS = 1.0e6


class Op:
    __slots__ = ("eng", "fn", "reads", "writes", "dma", "deps", "sig", "idx", "semref")

    def __init__(self, eng, fn, reads, writes, dma):
        self.eng = eng
        self.fn = fn
        self.reads = reads
        self.writes = writes
        self.dma = dma
        self.deps = []
        self.sig = False
        self.semref = None


def _freeze(fn):
    if fn is None or fn.__closure__ is None:
        return fn
    cells = []
    for c in fn.__closure__:
        try:
            cells.append(types.CellType(c.cell_contents))
        except ValueError:
            cells.append(c)
    return types.FunctionType(fn.__code__, fn.__globals__, fn.__name__, fn.__defaults__, tuple(cells))


class Prog:
    ENGS = ("pe", "dve", "act", "pool", "sp")

    def __init__(self, nc):
        self.nc = nc
        self.ops = []
        self.last_w = {}
        self.readers = {}
        self.since_bar = []

    def add(self, eng, fn, reads=(), writes=(), dma=False, extra_deps=()):
        reads = list(reads)
        writes = list(writes)
        for r in list(reads):
            if r.startswith("ps") and r[2:].isdigit():
                reads.remove(r)
                if r not in writes:
                    writes.append(r)
        fn = _freeze(fn)
        op = Op(eng, fn, tuple(reads), tuple(writes), dma)
        op.idx = len(self.ops)
        deps = set(extra_deps)
        for r in op.reads:
            w = self.last_w.get(r)
            if w is not None:
                deps.add(w)
        for w_ in op.writes:
            w = self.last_w.get(w_)
            if w is not None:
                deps.add(w)
            for rd in self.readers.get(w_, ()):
                deps.add(rd)
        deps.discard(op.idx)
        for r in op.reads:
            self.readers.setdefault(r, []).append(op.idx)
        for w_ in op.writes:
            self.last_w[w_] = op.idx
            self.readers[w_] = []
        op.deps = sorted(deps)
        self.ops.append(op)
        self.since_bar.append(op.idx)
        return op

    def pe(self, fn, reads=(), writes=()):
        return self.add("pe", fn, reads, writes)

    def dve(self, fn, reads=(), writes=()):
        return self.add("dve", fn, reads, writes)

    def act(self, fn, reads=(), writes=()):
        return self.add("act", fn, reads, writes)

    def pool(self, fn, reads=(), writes=()):
        return self.add("pool", fn, reads, writes)

    def dma(self, eng, fn, reads=(), writes=()):
        return self.add(eng, fn, reads, writes, dma=True)

    def reg(self, eng, value):
        if not hasattr(self, "_regs"):
            self._regs = {}
        if value not in self._regs:
            r = eng.alloc_register(f"c{value}")
            eng.reg_mov(r, value)
            self._regs[value] = r
        return self._regs[value]

    def barrier(self):
        prev = list(self.since_bar)
        last = {}
        dmas = []
        for i in prev:
            o = self.ops[i]
            if o.fn is None:
                continue
            if o.dma:
                dmas.append(i)
            else:
                last[o.eng] = i
        deps = dmas + list(last.values())
        self.since_bar = []
        for e in self.ENGS:
            self.add(e, None, extra_deps=deps)
        self.last_w = {}
        self.readers = {}

    def emit(self, max_ops=None):
        nc = self.nc
        if max_ops is not None:
            self.ops = self.ops[:max_ops]
        ops = self.ops

        def need_wait(dop, op):
            if dop.dma or op.dma:
                return True
            if dop.eng == "pe" and op.eng == "pe":
                return False
            return True

        for op in ops:
            if op.dma:
                op.sig = True
        for op in ops:
            for d in op.deps:
                if need_wait(ops[d], op):
                    ops[d].sig = True
        eng_sems = {e: [] for e in self.ENGS}
        eng_count = {e: 0 for e in self.ENGS}
        dma_sems = {e: [] for e in self.ENGS}
        dma_cnt = {}
        dma_rr = {e: 0 for e in self.ENGS}
        all_final = {}
        for op in ops:
            if not op.sig:
                continue
            if op.dma:
                lst = dma_sems[op.eng]
                k = dma_rr[op.eng] % N_DMA_SEMS
                dma_rr[op.eng] += 1
                if k >= len(lst):
                    lst.append(nc.alloc_semaphore(f"d_{op.eng}_{k}"))
                s = lst[k]
                c = dma_cnt.get(s.name, 0) + 1
                dma_cnt[s.name] = c
                op.semref = (s, 16 * c, 16)
            else:
                n = eng_count[op.eng]
                eng_count[op.eng] = n + 1
                ep = n // SEM_LIMIT
                lst = eng_sems[op.eng]
                if ep >= len(lst):
                    lst.append(nc.alloc_semaphore(f"e_{op.eng}_{ep}"))
                op.semref = (lst[ep], n % SEM_LIMIT + 1, 1)
            all_final[op.semref[0].name] = (op.semref[0], op.semref[1])
        per_eng = {e: [o for o in ops if o.eng == e] for e in self.ENGS}
        self.n_waits = 0

        def run_engine(ename, eng):
            seen = {}

            def wait(sem, val):
                if seen.get(sem.name, 0) >= val:
                    return
                seen[sem.name] = val
                eng.wait_ge(sem, val)
                self.n_waits += 1

            for op in per_eng[ename]:
                for d in op.deps:
                    dop = ops[d]
                    if dop.semref is None or not need_wait(dop, op):
                        continue
                    wait(dop.semref[0], dop.semref[1])
                if op.fn is None:
                    continue
                if op.dma:
                    s, v, inc = op.semref
                    if v > 16:
                        wait(s, v - 16)
                ins = op.fn(eng)
                if op.semref is not None:
                    ins.then_inc(op.semref[0], op.semref[2])
            if ename == "sp":
                for s, v in all_final.values():
                    wait(s, v)

        with nc.Block() as block:
            @block.tensor
            def _(e):
                run_engine("pe", e)

            @block.vector
            def _(e):
                run_engine("dve", e)

            @block.scalar
            def _(e):
                run_engine("act", e)

            @block.gpsimd
            def _(e):
                run_engine("pool", e)

            @block.sync
            def _(e):
                run_engine("sp", e)


_SPL = dict(gq=(0, 128), gk=(128, 256), gv=(256, 512), glr=(512, 528), gout=(528, 784),
            dq=(784, 1296), dk=(1296, 1808), dv=(1808, 2320), rq=(2320, 2576), rk=(2576, 2832),
            rv=(2832, 3088), rg=(3088, 3344))


def _swap_pairs(idx):
    idx = np.asarray(idx).reshape(-1, 2)
    return idx[:, ::-1].reshape(-1)


def win_col_perm():
    r = lambda k: np.arange(*_SPL[k])
    cols = []
    cols += [r("gq"), r("gk"), r("glr"), r("dq"), r("dk"), r("rq"), _swap_pairs(r("rq")),
             r("rk"), _swap_pairs(r("rk"))]
    cols += [r("gk"), r("gv"), r("gout"), r("rg"), r("dv"), r("rv")]
    return np.concatenate(cols)


FM_GQ, FM_GK, FM_GLR, FM_DQ, FM_DK, FM_RQ, FM_RQS, FM_RK, FM_RKS = 0, 128, 256, 272, 784, 1296, 1552, 1808, 2064
FM_END = 2320
TM0 = FM_END
TM1 = TM0 + 384
TM2 = TM1 + 512
TM3 = TM2 + 512
NCOL = TM3 + 256


def t5_bucket_np(n):
    n = np.asarray(n)
    max_exact = 16
    nf = np.maximum(n, 1).astype(np.float32)
    large = max_exact + (np.log(nf / max_exact) / math.log(128 / max_exact) * (32 - max_exact)).astype(np.int32)
    large = np.minimum(large, 31)
    return np.where(n < max_exact, n, large)


def host_consts(S, C):
    NT = S // 128
    c = {}
    c["ident"] = np.eye(128, dtype=np.float32)
    c["antiI"] = np.ascontiguousarray(np.eye(128, dtype=np.float32)[::-1])
    s = np.arange(128)[:, None]
    t = np.arange(128)[None, :]
    c["tri_incl"] = np.where(s <= t, -1.0 / 16, 0.0).astype(np.float32)
    c["tri_rev"] = np.where(s > t, -1.0 / 16, 0.0).astype(np.float32)
    c["maskT"] = (s <= t).astype(np.float32)
    c["lstrict"] = (s < t).astype(np.float32)
    c["ones"] = np.ones((128, 128), np.float32)
    r0 = np.zeros((128, 128), np.float32); r0[0, :] = 1.0
    c["row0"] = r0
    c["hmask3"] = (np.arange(128) >= 96).astype(np.float32).reshape(128, 1)
    c["hms4"] = ((np.arange(128)[:, None] // 32) == np.arange(4)[None, :]).astype(np.float32) * np.float32(32 ** -0.5)
    c["halfmask"] = ((np.arange(128)[:, None] // 64) == np.arange(2)[None, :]).astype(np.float32)
    log_g = np.log1p(-np.exp2(-5.0 - np.arange(4, dtype=np.float32))).astype(np.float32)
    rel = (t - s).astype(np.float32)
    DT = np.zeros((128, 4, 128), np.float32)
    for h in range(4):
        DT[:, h, :] = np.where(rel >= 0, np.exp(np.maximum(rel, 0) * log_g[h]), 0.0) * 0.125
    c["ret_DT"] = DT
    idx = np.arange(128, dtype=np.float32)
    c["ret_sd"] = (np.exp((127.0 - idx)[:, None] * log_g[None, :]) * 0.125).astype(np.float32)
    cd = np.zeros((128, 2, 128), np.float32)
    ch = np.zeros((128, 2), np.float32)
    for j in range(2):
        for half in range(2):
            h = 2 * j + half
            cd[64 * half:64 * half + 64, j, :] = np.exp((idx + 1.0) * log_g[h])[None, :]
            ch[64 * half:64 * half + 64, j] = np.exp(128.0 * log_g[h])
    c["ret_cdT"] = cd
    cdm = np.zeros((128, 2, 2, 128), np.float32)
    for half in range(2):
        cdm[64 * half:64 * half + 64, half, :, :] = cd[64 * half:64 * half + 64, :, :]
    c["ret_cdTm"] = cdm
    c["ret_chunk"] = ch
    pos = np.arange(S, dtype=np.float32)
    angle = (1.0 / (10000.0 ** np.linspace(0.0, 1.0, 32, dtype=np.float32))).astype(np.float32)
    angle = np.repeat(angle, 2)
    ang = (pos[:, None] * angle[None, :]).astype(np.float32)
    sin = np.sin(ang).astype(np.float32)
    cos = np.cos(ang).astype(np.float32)
    sgn = np.where(np.arange(64) % 2 == 0, -1.0, 1.0).astype(np.float32)
    cosT = np.concatenate([cos.T, cos.T], axis=0)
    sinsT = np.concatenate([(sin * sgn).T, (sin * sgn).T], axis=0)
    c["cosT"] = np.ascontiguousarray(cosT)
    c["sinsT"] = np.ascontiguousarray(sinsT)
    OH = np.zeros((33, 1152), np.float32)
    for i in range(1151):
        dist = i - 511
        if dist < 0:
            OH[32, i] = 1.0
        else:
            OH[int(t5_bucket_np(dist)), i] = 1.0
    c["t5oh"] = OH
    c["ecap"] = np.tile((np.arange(32, dtype=np.float32) * C + 1.0)[None, :], (128, 1))
    ts = np.zeros((128, NT, 4, 2), np.int32)
    tok = np.arange(128)[:, None] + 128 * np.arange(NT)[None, :]
    for j in range(4):
        ts[:, :, j, 0] = tok
        ts[:, :, j, 1] = 4 * tok + j
    c["tokslot"] = ts.reshape(128, NT * 8)
    li = np.zeros((32 * C, 2), np.int32)
    li[:, 0] = S
    li[:, 1] = 1 << 24
    c["list_init"] = li
    return c


CONST_SHAPES = None


def build(S, L, C, debug=(), lam_inits=None, stop=None, max_ops=None):
    NT = S // 128
    SC = min(512, S)
    NSC = S // SC
    TPS = SC // 128
    NR = C // 128
    nc = bass.Bass("TRN2", target_bir_lowering=False)
    P = Prog(nc)
    if lam_inits is None:
        lam_inits = [0.8 - 0.6 * math.exp(-0.3 * i) for i in range(L)]

    def din(name, shape, dt=F32):
        return nc.dram_tensor(name, list(shape), dt, kind="ExternalInput").ap()

    def dscr(name, shape, dt=F32):
        kind = "ExternalOutput" if name in debug else "Internal"
        return nc.dram_tensor(name, list(shape), dt, kind=kind).ap()

    x_in = din("x", [S, D])
    p_in = din("p", [L, S, PLE])
    w_in = din("w_in", [L, D, NCOL])
    wg_aug = din("wg_aug", [L, 17, 128])
    gng_in = din("gla_norm_g", [L, 64])
    dlam_in = din("diff_lambda", [L, 4, 64])
    dng_in = din("diff_norm_g", [L, 128])
    w_out = din("w_out", [L, D, D])
    rb_aug = din("rb_aug", [33, 4])
    ln1g = din("ln1_g", [L, D])
    ln1b = din("ln1_b", [L, D])
    w_router = din("w_router", [L, D, NE])
    b_router = din("b_router", [L, NE])
    w_gu = din("w_gu", [L, NE, D, 2 * D])
    b_gu = din("b_gu", [L, 128, NE, 16])
    w_down = din("w_down", [L, NE, D, D])
    b_down = din("b_down", [L, NE, D])
    w_pg = din("w_pg", [L, D, D])
    b_pg = din("b_pg", [L, D])
    w_pp = din("w_pp", [L, PLE, D])
    ln2g = din("ln2_g", [L, D])
    ln2b = din("ln2_b", [L, D])
    hc = host_consts(S, C)
    cin = {}
    for k, v in hc.items():
        cin[k] = din("c_" + k, v.shape, I32 if v.dtype == np.int32 else F32)
    out = nc.dram_tensor("out", [S, D], F32, kind="ExternalOutput").ap()

    xbuf = [dscr(f"xbuf{i}", [S, D]) for i in range(2)]
    zfm_f = dscr("zfm_f", [272, S])
    zfm_b = dscr("zfm_b", [1536, S], BF16)
    ztm_f = dscr("ztm_f", [S, 640])
    ztm_b = dscr("ztm_b", [S, 1024], BF16)
    mixed = dscr("mixed", [S, D], BF16)
    h_bf = dscr("h_bf", [S + 1, D], BF16)
    r2 = dscr("r2", [S, D])
    lst = dscr("lst", [NE * C, 2], I32)
    ybuf = dscr("ybuf", [4 * S, D])
    Fd = dscr("Fd", [4, 1152])

    SB_LIMIT = 229376 - 512
    sb_state = {"off": 16640, "n": 0, "base": 16640}

    def sb(name, shape, dt):
        esz = {F32: 4, BF16: 2, I32: 4}[dt]
        nbytes = int(np.prod(shape[1:])) * esz
        nbytes = (nbytes + 63) // 64 * 64
        off = sb_state["off"]
        assert off + nbytes <= SB_LIMIT, (name, off, nbytes)
        sb_state["off"] = off + nbytes
        sb_state["n"] += 1
        return nc.alloc_sbuf_tensor_at(f"{name}_{sb_state['n']}", list(shape), dt, offset=off)

    import os as _os
    def phase_begin():
        P.barrier()
        sb_state["pc"] = sb_state.get("pc", 0) + 1
        if not (_os.environ.get("NO_SB_RESET") and sb_state["pc"] == int(_os.environ.get("NO_SB_RESET"))):
            sb_state["off"] = sb_state["base"]

    ps = [nc.alloc_psum_tensor(f"ps{i}", [128, 512], F32) for i in range(8)]

    def PS(i):
        return f"ps{i}"

    ident_f = sb("ident_f", [128, 128], F32)
    ident_b = sb("ident_b", [128, 128], BF16)
    anti_f = sb("anti_f", [128, 128], F32)
    anti_b = sb("anti_b", [128, 128], BF16)
    ones_f = sb("ones_f", [128, 128], F32)
    ones_b = sb("ones_b", [128, 128], BF16)
    row0_f = sb("row0_f", [128, 128], F32)
    row0_b = sb("row0_b", [128, 128], BF16)
    tri_incl = sb("tri_incl", [128, 128], F32)
    tri_rev = sb("tri_rev", [128, 128], F32)
    maskT = sb("maskT", [128, 128], F32)
    lstrict = sb("lstrict", [128, 128], F32)
    ret_DT = sb("ret_DT", [128, 4, 128], F32)
    ret_sd = sb("ret_sd", [128, 4], F32)
    ret_cdT = sb("ret_cdT", [128, 2, 128], F32)
    ret_chunk = sb("ret_chunk", [128, 2], F32)
    ecap = sb("ecap", [128, 32], F32)
    hmask3 = sb("hmask3", [128, 1], F32)
    hms4 = sb("hms4", [128, 4], F32)
    halfmask = sb("halfmask", [128, 2], F32)
    ret_cdTm = sb("ret_cdTm", [128, 2, 2, 128], F32)
    tokslot = sb("tokslot", [128, NT * 8], I32)
    cfar = sb("cfar", [128, 4], F32)
    BTb = sb("BTb", [128, 4, 1024], BF16)
    g4all = sb("g4all", [128, NT, 4], F32)
    zero_b = sb("zero_b", [128, 1024], BF16)
    for nm, t_ in (("row0", row0_f), ("ident", ident_f), ("antiI", anti_f), ("ones", ones_f), ("tri_incl", tri_incl), ("tri_rev", tri_rev),
                   ("maskT", maskT), ("lstrict", lstrict), ("ret_DT", ret_DT), ("ret_sd", ret_sd),
                   ("ret_cdT", ret_cdT), ("ret_chunk", ret_chunk), ("ecap", ecap), ("hmask3", hmask3), ("hms4", hms4), ("halfmask", halfmask), ("ret_cdTm", ret_cdTm), ("tokslot", tokslot)):
        P.dma("sp", lambda e, nm=nm, t_=t_: e.dma_start(out=t_[:], in_=cin[nm]), writes=[nm])
    P.dve(lambda e: e.tensor_copy(out=ident_b[:], in_=ident_f[:]), reads=["ident"], writes=["ident_b"])
    P.dve(lambda e: e.tensor_copy(out=ones_b[:], in_=ones_f[:]), reads=["ones"], writes=["ones_b"])
    P.dve(lambda e: e.tensor_copy(out=row0_b[:], in_=row0_f[:]), reads=["row0"], writes=["row0_b"])
    P.dve(lambda e: e.tensor_copy(out=anti_b[:], in_=anti_f[:]), reads=["antiI"], writes=["anti_b"])
    P.dve(lambda e: e.memset(zero_b[:], 0.0), writes=["zero_b"])
    P.dma("sp", lambda e: e.dma_start(out=h_bf[S:S + 1, :], in_=zero_b[0:1, :]), reads=["zero_b"], writes=["h_bf_z"])
    sb_state["base"] = sb_state["off"]
    zero_f = sb("zero_f", [128, 2048], F32)
    BTf = sb("BTf", [128, 4, 1024], F32)
    P.dve(lambda e: e.memset(zero_f[:], 0.0), writes=["zero_f"])
    ybv = ybuf.rearrange("(a p r) d -> a p (r d)", p=128, r=2)
    for a in range(ybv.shape[0]):
        P.dma("sp" if a % 2 == 0 else "pool", lambda e, a=a: e.dma_start(out=ybv[a], in_=zero_f[:]),
              reads=["zero_f"], writes=[f"ybz{a}"])
    t5oh = sb("t5oh", [33, 1152], F32)
    rbs = sb("rbs", [33, 4], F32)
    Fs = sb("Fs", [4, 1152], F32)
    P.dma("sp", lambda e: e.dma_start(out=t5oh[:], in_=cin["t5oh"]), writes=["t5oh"])
    P.dma("sp", lambda e: e.dma_start(out=rbs[:], in_=rb_aug), writes=["rbs"])
    for i, (lo, n) in enumerate(((0, 512), (512, 512), (1024, 128))):
        P.pe(lambda e, i=i, lo=lo, n=n: e.matmul(ps[i][0:4, 0:n], lhsT=rbs[:, :], rhs=t5oh[:, lo:lo + n], start=True, stop=True),
             reads=["t5oh", "rbs"], writes=[PS(i)])
        P.dve(lambda e, i=i, lo=lo, n=n: e.tensor_copy(out=Fs[:, lo:lo + n], in_=ps[i][0:4, 0:n]), reads=[PS(i)], writes=["Fs"])
    P.dma("sp", lambda e: e.dma_start(out=Fd, in_=Fs[:]), reads=["Fs"], writes=["Fd"])
    for h in range(4):
        src = bass.AP(tensor=Fd.tensor, offset=h * 1152, ap=[[1, 128], [1, 1024]])
        P.dma("sp", lambda e, h=h, src=src: e.dma_start(out=BTf[:, h, :], in_=src), reads=["Fd"], writes=["BTf"])
    P.dve(lambda e: e.tensor_copy(out=BTb[:], in_=BTf[:]), reads=["BTf"], writes=["BTb"])
    P.dve(lambda e: e.tensor_copy(out=cfar[:], in_=BTf[:, :, 1023]), reads=["BTf"], writes=["cfar"])

    def rms_scale(o_ps_ap, nh, hd, sq, ss, rstd):
        pass

    def layer_norm(src, dst, gam, bet, tagp, st6, mv, lrs, xn):
        sname, dname = tagp
        for hb in range(2):
            P.dve(lambda e, hb=hb: e.bn_stats(out=st6[:, hb * 6:(hb + 1) * 6], in_=src[:, hb * 512:(hb + 1) * 512]),
                  reads=[sname], writes=["st6"])
        P.dve(lambda e: e.bn_aggr(out=mv[:], in_=st6[:]), reads=["st6"], writes=["mv"])
        P.dve(lambda e: e.tensor_scalar(out=lrs[:], in0=mv[:, 1:2], scalar1=LN_EPS, scalar2=None, op0=ALU.add),
              reads=["mv"], writes=["lrs"])
        P.act(lambda e: e.activation(out=lrs[:], in_=lrs[:], func=AF.Sqrt), reads=["lrs"], writes=["lrs"])
        P.dve(lambda e: e.reciprocal(out=lrs[:], in_=lrs[:]), reads=["lrs"], writes=["lrs"])
        P.dve(lambda e: e.tensor_scalar(out=xn[:], in0=src[:], scalar1=mv[:, 0:1], scalar2=lrs[:, 0:1], op0=ALU.subtract, op1=ALU.mult),
              reads=[sname, "mv", "lrs"], writes=["xn"])
        P.pool(lambda e: e.tensor_tensor(out=xn[:], in0=xn[:], in1=gam[:], op=ALU.mult), reads=["xn", "lng"], writes=["xn"])
        P.pool(lambda e: e.tensor_tensor(out=dst[:], in0=xn[:], in1=bet[:], op=ALU.add), reads=["xn", "lnb"], writes=[dname])

    def layer(l):
        X_in = x_in if l == 0 else xbuf[(l - 1) % 2]
        X_out = out if l == L - 1 else xbuf[l % 2]
        lam_init = lam_inits[l]
        if stop == ("pre", l):
            return True

        def _phase1():
            phase_begin()
            xT = sb("xT", [128, 8, S], BF16)
            win = sb("win", [128, 8, NCOL], BF16)
            xin = [sb(f"xin{i}", [128, D], F32) for i in range(2)]
            stf = [sb(f"stf{i}", [128, SC], F32) for i in range(4)]
            stb = [sb(f"stb{i}", [128, SC], BF16) for i in range(4)]
            cosb = [sb(f"cosb{i}", [128, SC], F32) for i in range(2)]
            sinb = [sb(f"sinb{i}", [128, SC], F32) for i in range(2)]
            rt1 = [sb(f"rt1{i}", [128, SC], F32) for i in range(2)]
            rt2 = [sb(f"rt2{i}", [128, SC], F32) for i in range(2)]
            tmf = [sb(f"tmf{i}", [128, 640], F32) for i in range(2)]
            tmb = [sb(f"tmb{i}", [128, 1024], BF16) for i in range(2)]
            for k in range(8):
                P.dma("pool", lambda e, k=k: e.dma_start(out=win[:, k, :], in_=w_in[l, k * 128:(k + 1) * 128, :]),
                      writes=[f"win{k}"])
            WIN = [f"win{k}" for k in range(8)]
            for t in range(NT):
                xb = xin[t % 2]
                P.dma("sp", lambda e, t=t, xb=xb: e.dma_start(out=xb[:], in_=X_in[t * 128:(t + 1) * 128, :]),
                      writes=[f"xin{t % 2}"])
                for hb in range(2):
                    bank = (t % 2) * 2 + hb
                    for kk in range(4):
                        k = hb * 4 + kk
                        P.pe(lambda e, bank=bank, kk=kk, k=k, xb=xb: e.matmul(
                            ps[bank][:, kk * 128:(kk + 1) * 128], lhsT=xb[:, k * 128:(k + 1) * 128], rhs=ident_f[:],
                            start=True, stop=True), reads=[f"xin{t % 2}", "ident"], writes=[PS(bank)])
                    eng = P.act if hb == 0 else P.dve
                    if hb == 0:
                        P.act(lambda e, bank=bank, t=t, hb=hb: e.activation(
                            out=xT[:, hb * 4:hb * 4 + 4, t * 128:(t + 1) * 128],
                            in_=ps[bank][:, :].rearrange("p (k n) -> p k n", k=4), func=AF.Copy),
                            reads=[PS(bank)], writes=[f"xT{t // TPS}"])
                    else:
                        P.dve(lambda e, bank=bank, t=t, hb=hb: e.tensor_copy(
                            out=xT[:, hb * 4:hb * 4 + 4, t * 128:(t + 1) * 128],
                            in_=ps[bank][:, :].rearrange("p (k n) -> p k n", k=4)),
                            reads=[PS(bank)], writes=[f"xT{t // TPS}"])
            bankc = [0]

            def nb():
                b = bankc[0] % 8
                bankc[0] += 1
                return b

            stc = [0]
            for sc in range(NSC):
                tok = slice(sc * SC, (sc + 1) * SC)
                XT = [f"xT{sc}"]

                def fm_mm(col0, ncols, bank):
                    for k in range(8):
                        P.pe(lambda e, k=k, col0=col0, ncols=ncols, bank=bank: e.matmul(
                            ps[bank][0:ncols, 0:SC], lhsT=win[:, k, col0:col0 + ncols], rhs=xT[:, k, tok],
                            start=(k == 0), stop=(k == 7)), reads=XT + [f"win{k}"], writes=[PS(bank)])

                for (col0, ncols, row0) in ((FM_GQ, 128, 0), (FM_GK, 128, 128), (FM_GLR, 16, 256)):
                    bank = nb()
                    fm_mm(col0, 128, bank)
                    si = stc[0] % 4
                    stc[0] += 1
                    P.act(lambda e, bank=bank, ncols=ncols, si=si: e.activation(out=stf[si][0:ncols, :], in_=ps[bank][0:ncols, 0:SC], func=AF.Copy),
                          reads=[PS(bank)], writes=[f"stf{si}"])
                    P.dma("sp", lambda e, si=si, ncols=ncols, row0=row0: e.dma_start(out=zfm_f[row0:row0 + ncols, tok], in_=stf[si][0:ncols, :]),
                          reads=[f"stf{si}"], writes=[f"zfm_f{sc}"])
                for i in range(8):
                    col0 = FM_DQ + i * 128
                    bank = nb()
                    fm_mm(col0, 128, bank)
                    si = stc[0] % 4
                    stc[0] += 1
                    scale = 0.125 if i < 4 else 1.0
                    if i % 2 == 0:
                        P.act(lambda e, bank=bank, si=si, scale=scale: e.activation(out=stb[si][:, :], in_=ps[bank][:, 0:SC], func=AF.Copy, scale=scale),
                              reads=[PS(bank)], writes=[f"stb{si}"])
                    else:
                        P.dve(lambda e, bank=bank, si=si, scale=scale: e.tensor_scalar(out=stb[si][:, :], in0=ps[bank][:, 0:SC], scalar1=scale, scalar2=None, op0=ALU.mult),
                              reads=[PS(bank)], writes=[f"stb{si}"])
                    P.dma("sp", lambda e, si=si, i=i: e.dma_start(out=zfm_b[i * 128:(i + 1) * 128, tok], in_=stb[si][:, :]),
                          reads=[f"stb{si}"], writes=[f"zfm_b{sc}"])
                cb = sc % 2
                P.dma("sp", lambda e, cb=cb: e.dma_start(out=cosb[cb][:], in_=cin["cosT"][:, tok]), writes=[f"cosb{cb}"])
                P.dma("sp", lambda e, cb=cb: e.dma_start(out=sinb[cb][:], in_=cin["sinsT"][:, tok]), writes=[f"sinb{cb}"])
                ri = 0
                for (c_a, c_s, row0) in ((FM_RQ, FM_RQS, 1024), (FM_RK, FM_RKS, 1280)):
                    for j in range(2):
                        b1 = nb()
                        fm_mm(c_a + j * 128, 128, b1)
                        b2 = nb()
                        fm_mm(c_s + j * 128, 128, b2)
                        r_ = ri % 2
                        ri += 1
                        si = stc[0] % 4
                        stc[0] += 1
                        P.dve(lambda e, b1=b1, r_=r_, cb=cb: e.tensor_tensor(out=rt1[r_][:], in0=ps[b1][:, 0:SC], in1=cosb[cb][:], op=ALU.mult),
                              reads=[PS(b1), f"cosb{cb}"], writes=[f"rt1{r_}"])
                        P.dve(lambda e, b2=b2, r_=r_, cb=cb: e.tensor_tensor(out=rt2[r_][:], in0=ps[b2][:, 0:SC], in1=sinb[cb][:], op=ALU.mult),
                              reads=[PS(b2), f"sinb{cb}"], writes=[f"rt2{r_}"])
                        P.pool(lambda e, r_=r_, si=si: e.tensor_tensor(out=stb[si][:], in0=rt1[r_][:], in1=rt2[r_][:], op=ALU.add),
                               reads=[f"rt1{r_}", f"rt2{r_}"], writes=[f"stb{si}"])
                        P.dma("sp", lambda e, si=si, row0=row0, j=j: e.dma_start(out=zfm_b[row0 + j * 128:row0 + (j + 1) * 128, tok], in_=stb[si][:, :]),
                              reads=[f"stb{si}"], writes=[f"zfm_b{sc}"])
                for tt in range(TPS):
                    t = sc * TPS + tt
                    tsl = slice(t * 128, (t + 1) * 128)
                    ti = t % 2
                    banks = []
                    for (col0, ncols) in ((TM0, 384), (TM1, 512), (TM2, 512), (TM3, 256)):
                        bank = nb()
                        banks.append(bank)
                        for k in range(8):
                            P.pe(lambda e, k=k, col0=col0, ncols=ncols, bank=bank, tsl=tsl: e.matmul(
                                ps[bank][:, 0:ncols], lhsT=xT[:, k, tsl], rhs=win[:, k, col0:col0 + ncols],
                                start=(k == 0), stop=(k == 7)), reads=XT + [f"win{k}"], writes=[PS(bank)])
                    b0, b1, b2, b3 = banks
                    P.act(lambda e, b0=b0, ti=ti: e.activation(out=tmf[ti][:, 0:128], in_=ps[b0][:, 0:128], func=AF.Copy),
                          reads=[PS(b0)], writes=[f"tmf{ti}"])
                    P.dve(lambda e, b0=b0, ti=ti: e.tensor_copy(out=tmb[ti][:, 0:256], in_=ps[b0][:, 128:384]),
                          reads=[PS(b0)], writes=[f"tmb{ti}"])
                    P.act(lambda e, b1=b1, ti=ti: e.activation(out=tmf[ti][:, 128:640], in_=ps[b1][:, 0:512], func=AF.Sigmoid),
                          reads=[PS(b1)], writes=[f"tmf{ti}"])
                    P.dve(lambda e, b1=b1, ti=ti: e.tensor_tensor(out=tmf[ti][:, 128:640], in0=tmf[ti][:, 128:640], in1=ps[b1][:, 0:512], op=ALU.mult),
                          reads=[PS(b1), f"tmf{ti}"], writes=[f"tmf{ti}"])
                    P.dve(lambda e, b2=b2, ti=ti: e.tensor_copy(out=tmb[ti][:, 256:768], in_=ps[b2][:, 0:512]),
                          reads=[PS(b2)], writes=[f"tmb{ti}"])
                    P.act(lambda e, b3=b3, ti=ti: e.activation(out=tmb[ti][:, 768:1024], in_=ps[b3][:, 0:256], func=AF.Copy),
                          reads=[PS(b3)], writes=[f"tmb{ti}"])
                    P.dma("sp", lambda e, ti=ti, tsl=tsl: e.dma_start(out=ztm_f[tsl, :], in_=tmf[ti][:]), reads=[f"tmf{ti}"], writes=[f"ztm_f{t}"])
                    P.dma("sp", lambda e, ti=ti, tsl=tsl: e.dma_start(out=ztm_b[tsl, :], in_=tmb[ti][:]), reads=[f"tmb{ti}"], writes=[f"ztm_b{t}"])
        _phase1()
        if stop == ("inproj", l):
            return True

        def _phase2():
            phase_begin()
            gqT = sb("gqT", [128, S], F32)
            gkT = sb("gkT", [128, S], F32)
            glra = sb("glra", [128, S], F32)
            gk_tm = sb("gk_tm", [128, NT, 128], F32)
            gv = sb("gv", [128, NT, 256], BF16)
            gsil = sb("gsil", [128, NT, 256], F32)
            mixg = sb("mixg", [128, NT, 256], BF16)
            wga = sb("wga", [128, 128], F32)
            gng = sb("gng", [128, 64], F32)
            Sf = sb("Sf", [128, 256], F32)
            Sb = sb("Sb", [128, 256], BF16)
            e1 = sb("e1", [128, 128], F32)
            spt = sb("spt", [128, 128], F32)
            eq = sb("eq", [128, 128], F32)
            ek = sb("ek", [128, 128], F32)
            er = sb("er", [128, 128], F32)
            qp = sb("qp", [128, 128], BF16)
            kp = sb("kp", [128, 128], BF16)
            kpp = sb("kpp", [128, 128], BF16)
            qpm = sb("qpm", [128, 4, 128], BF16)
            AT = sb("AT", [128, 4, 128], BF16)
            sq = sb("sq", [128, 256], F32)
            ss = sb("ss", [128, 4], F32)
            rstd = sb("rstd", [128, 4], F32)
            on = sb("on", [128, 4, 64], F32)
            on2 = sb("on2", [128, 4, 64], F32)
            P.dve(lambda e: e.memset(glra[:, :], 0.0), writes=["glra"])
            P.dve(lambda e: e.memset(glra[0:32, :], 1.0), writes=["glra"])
            P.dve(lambda e: e.memset(wga[:, :], 0.0), writes=["wga"])
            P.dma("sp", lambda e: e.dma_start(out=gqT[:], in_=zfm_f[0:128, :]), writes=["gqT"])
            P.dma("sp", lambda e: e.dma_start(out=gkT[:], in_=zfm_f[128:256, :]), writes=["gkT"])
            P.dma("sp", lambda e: e.dma_start(out=glra[0:16, :], in_=zfm_f[256:272, :]), writes=["glra"])
            P.dma("sp", lambda e: e.dma_start(out=gk_tm[:], in_=ztm_f[:, 0:128].rearrange("(t p) c -> p t c", p=128)), writes=["gk_tm"])
            P.dma("sp", lambda e: e.dma_start(out=gsil[:], in_=ztm_f[:, 128:384].rearrange("(t p) c -> p t c", p=128)), writes=["gsil"])
            P.dma("sp", lambda e: e.dma_start(out=gv[:], in_=ztm_b[:, 0:256].rearrange("(t p) c -> p t c", p=128)), writes=["gv"])
            P.dma("sp", lambda e: e.dma_start(out=wga[0:17, :], in_=wg_aug[l]), writes=["wga"])
            P.dma("sp", lambda e: e.dma_start(out=gng[:], in_=gng_in[l:l + 1, :].partition_broadcast(128)), writes=["gng"])
            P.dve(lambda e: e.memset(Sf[:], 0.0), writes=["Sf"])
            P.dve(lambda e: e.memset(Sb[:], 0.0), writes=["Sb"])
            for c in range(NT):
                ck = slice(c * 128, (c + 1) * 128)
                P.pe(lambda e, ck=ck: e.matmul(ps[0][:, 0:128], lhsT=glra[:, ck], rhs=wga[:, :], start=True, stop=True),
                     reads=["glra", "wga"], writes=[PS(0)])
                P.act(lambda e: e.activation(out=e1[:], in_=ps[0][:, 0:128], func=AF.Exp, scale=-1.0), reads=[PS(0)], writes=["e1"])
                P.act(lambda e: e.activation(out=spt[:], in_=e1[:], func=AF.Ln, bias=1.0), reads=["e1"], writes=["spt"])
                P.pe(lambda e: e.matmul(ps[1][:, 0:128], lhsT=spt[:], rhs=tri_incl[:], start=True, stop=True),
                     reads=["spt", "tri_incl"], writes=[PS(1)])
                P.pe(lambda e: e.matmul(ps[1][:, 128:256], lhsT=tri_rev[:], rhs=spt[:], start=True, stop=True),
                     reads=["spt", "tri_rev"], writes=[PS(1)])
                P.act(lambda e: e.activation(out=eq[:], in_=ps[1][:, 0:128], func=AF.Exp), reads=[PS(1)], writes=["eq"])
                P.act(lambda e: e.activation(out=ek[:], in_=ps[1][:, 0:128], func=AF.Exp, scale=-1.0), reads=[PS(1)], writes=["ek"])
                P.act(lambda e: e.activation(out=er[:], in_=ps[1][:, 128:256], func=AF.Exp), reads=[PS(1)], writes=["er"])
                for h in range(4):
                    P.dve(lambda e, ck=ck, h=h: e.scalar_tensor_tensor(out=qpm[:, h, :], in0=gqT[:, ck], scalar=hms4[:, h:h + 1], in1=eq[:],
                                                                     op0=ALU.mult, op1=ALU.mult), reads=["gqT", "eq", "hms4"], writes=["qpm"])
                P.dve(lambda e, ck=ck: e.tensor_tensor(out=kp[:], in0=gkT[:, ck], in1=ek[:], op=ALU.mult), reads=["gkT", "ek"], writes=["kp"])
                P.dve(lambda e, c=c: e.tensor_tensor(out=kpp[:], in0=gk_tm[:, c, :], in1=er[:], op=ALU.mult), reads=["gk_tm", "er"], writes=["kpp"])
                for h in range(4):
                    P.pe(lambda e, h=h: e.matmul(ps[2][:, h * 128:(h + 1) * 128], lhsT=kp[:, :], rhs=qpm[:, h, :],
                                                start=True, stop=True), reads=["kp", "qpm"], writes=[PS(2)])
                P.dve(lambda e: e.tensor_tensor(out=AT[:], in0=ps[2][:, :].rearrange("p (h t) -> p h t", h=4),
                                                in1=maskT[:, :].unsqueeze(1).to_broadcast([128, 4, 128]), op=ALU.mult),
                      reads=[PS(2), "maskT"], writes=["AT"])
                P.pe(lambda e, c=c: e.matmul(ps[3][:, 0:256], lhsT=kpp[:], rhs=gv[:, c, :], start=True, stop=True),
                     reads=["kpp", "gv"], writes=[PS(3)])
                for h in range(4):
                    P.pe(lambda e, h=h, c=c: e.matmul(ps[4][:, h * 64:(h + 1) * 64], lhsT=AT[:, h, :], rhs=gv[:, c, h * 64:(h + 1) * 64],
                                                      start=True, stop=False), reads=["AT", "gv"], writes=[PS(4)])
                    P.pe(lambda e, h=h: e.matmul(ps[4][:, h * 64:(h + 1) * 64], lhsT=qpm[:, h, :],
                                                rhs=Sb[:, 64 * h:64 * h + 64], start=False, stop=True),
                         reads=["qpm", "Sb"], writes=[PS(4)])
                P.dve(lambda e: e.scalar_tensor_tensor(out=Sf[:], in0=Sf[:], scalar=eq[:, 127:128], in1=ps[3][:, 0:256], op0=ALU.mult, op1=ALU.add),
                      reads=["Sf", "eq", PS(3)], writes=["Sf"])
                P.act(lambda e: e.activation(out=Sb[:], in_=Sf[:], func=AF.Copy), reads=["Sf"], writes=["Sb"])
                P.act(lambda e: e.activation(out=sq[:], in_=ps[4][:, 0:256], func=AF.Square), reads=[PS(4)], writes=["sq"])
                P.dve(lambda e: e.tensor_reduce(out=ss[:], in_=sq[:, :].rearrange("p (h v) -> p h v", h=4), axis=AX.X, op=ALU.add),
                      reads=["sq"], writes=["ss"])
                P.dve(lambda e: e.tensor_scalar(out=rstd[:], in0=ss[:], scalar1=1.0 / 64, scalar2=HN_EPS, op0=ALU.mult, op1=ALU.add),
                      reads=["ss"], writes=["rstd"])
                P.act(lambda e: e.activation(out=rstd[:], in_=rstd[:], func=AF.Sqrt), reads=["rstd"], writes=["rstd"])
                P.dve(lambda e: e.reciprocal(out=rstd[:], in_=rstd[:]), reads=["rstd"], writes=["rstd"])
                P.dve(lambda e: e.tensor_tensor(out=on[:], in0=ps[4][:, 0:256].rearrange("p (h v) -> p h v", h=4),
                                                in1=rstd[:, :].unsqueeze(2).to_broadcast([128, 4, 64]), op=ALU.mult),
                      reads=[PS(4), "rstd"], writes=["on"])
                P.pool(lambda e: e.tensor_tensor(out=on2[:], in0=on[:], in1=gng[:, :].unsqueeze(1).to_broadcast([128, 4, 64]), op=ALU.mult),
                       reads=["on", "gng"], writes=["on2"])
                P.pool(lambda e, c=c: e.tensor_tensor(out=mixg[:, c, :], in0=on2[:, :, :].rearrange("p h v -> p (h v)"), in1=gsil[:, c, :], op=ALU.mult),
                       reads=["on2", "gsil"], writes=["mixg"])
            P.dma("sp", lambda e: e.dma_start(out=mixed[:, 0:256].rearrange("(t p) c -> p t c", p=128), in_=mixg[:]),
                  reads=["mixg"], writes=["mixed_g"])
        _phase2()
        if stop == ("gla", l):
            return True

        def _phase3():
            phase_begin()
            rqT = sb("rqT", [128, 2, S], BF16)
            rkT = sb("rkT", [128, 2, S], BF16)
            rv = sb("rv", [128, NT, 256], BF16)
            rsil = sb("rsil", [128, NT, 256], F32)
            mixr = sb("mixr", [128, NT, 256], BF16)
            RSf = sb("RSf", [128, 2, 128], F32)
            RSb = sb("RSb", [128, 2, 128], BF16)
            RAT = sb("RAT", [128, 4, 128], BF16)
            kd = sb("kd", [128, 4, 64], BF16)
            rqm = sb("rqm", [128, 2, 2, 128], BF16)
            rqcm = sb("rqcm", [128, 2, 2, 128], BF16)
            sq = sb("rsq", [128, 256], F32)
            ss = sb("rss", [128, 4], F32)
            rstd = sb("rrstd", [128, 4], F32)
            on = sb("ron", [128, 4, 64], F32)
            for j in range(2):
                P.dma("sp", lambda e, j=j: e.dma_start(out=rqT[:, j, :], in_=zfm_b[1024 + j * 128:1024 + (j + 1) * 128, :]), writes=["rqT"])
                P.dma("sp", lambda e, j=j: e.dma_start(out=rkT[:, j, :], in_=zfm_b[1280 + j * 128:1280 + (j + 1) * 128, :]), writes=["rkT"])
            P.dma("sp", lambda e: e.dma_start(out=rv[:], in_=ztm_b[:, 768:1024].rearrange("(t p) c -> p t c", p=128)), writes=["rv"])
            P.dma("sp", lambda e: e.dma_start(out=rsil[:], in_=ztm_f[:, 384:640].rearrange("(t p) c -> p t c", p=128)), writes=["rsil"])
            P.dve(lambda e: e.memset(RSf[:], 0.0), writes=["RSf"])
            P.dve(lambda e: e.memset(RSb[:], 0.0), writes=["RSb"])
            for c in range(NT):
                ck = slice(c * 128, (c + 1) * 128)
                for half in range(2):
                    P.dve(lambda e, half=half, ck=ck: e.tensor_scalar(out=rqm[:, half, :, :], in0=rqT[:, :, ck], scalar1=halfmask[:, half:half + 1],
                                                                     scalar2=None, op0=ALU.mult), reads=["rqT", "halfmask"], writes=["rqm"])
                    P.dve(lambda e, half=half, ck=ck: e.tensor_tensor(out=rqcm[:, half, :, :], in0=rqT[:, :, ck], in1=ret_cdTm[:, half, :, :], op=ALU.mult),
                          reads=["rqT", "ret_cdTm"], writes=["rqcm"])
                for h in range(4):
                    j, half = h // 2, h % 2
                    pr = slice(64 * half, 64 * half + 64)
                    P.pe(lambda e, h=h, j=j, half=half, ck=ck: e.matmul(ps[0][:, h * 128:(h + 1) * 128], lhsT=rkT[:, j, ck], rhs=rqm[:, half, j, :],
                                                                     start=True, stop=True), reads=["rkT", "rqm"], writes=[PS(0)])
                P.dve(lambda e: e.tensor_tensor(out=RAT[:], in0=ps[0][:, :].rearrange("p (h t) -> p h t", h=4), in1=ret_DT[:], op=ALU.mult),
                      reads=[PS(0), "ret_DT"], writes=["RAT"])
                for j in range(2):
                    P.pe(lambda e, j=j, ck=ck: e.matmul(ps[1][:, j * 128:(j + 1) * 128], lhsT=rkT[:, j, ck], rhs=ident_b[:], start=True, stop=True),
                         reads=["rkT", "ident_b"], writes=[PS(1)])
                P.dve(lambda e: e.tensor_tensor(out=kd[:], in0=ps[1][:, 0:256].rearrange("p (h d) -> p h d", h=4),
                                                in1=ret_sd[:, :].unsqueeze(2).to_broadcast([128, 4, 64]), op=ALU.mult),
                      reads=[PS(1), "ret_sd"], writes=["kd"])
                for j in range(2):
                    P.pe(lambda e, j=j, c=c: e.matmul(ps[2][:, j * 128:(j + 1) * 128], lhsT=kd[:, 2 * j:2 * j + 2, :].rearrange("p h d -> p (h d)"),
                                                      rhs=rv[:, c, j * 128:(j + 1) * 128], start=True, stop=True),
                         reads=["kd", "rv"], writes=[PS(2)])
                for h in range(4):
                    j, half = h // 2, h % 2
                    pr = slice(64 * half, 64 * half + 64)
                    P.pe(lambda e, h=h, c=c: e.matmul(ps[3][:, h * 64:(h + 1) * 64], lhsT=RAT[:, h, :], rhs=rv[:, c, h * 64:(h + 1) * 64],
                                                      start=True, stop=False), reads=["RAT", "rv"], writes=[PS(3)])
                    P.pe(lambda e, h=h, j=j, half=half: e.matmul(ps[3][:, h * 64:(h + 1) * 64], lhsT=rqcm[:, half, j, :],
                                                              rhs=RSb[:, j, 64 * half:64 * half + 64], start=False, stop=True),
                         reads=["rqcm", "RSb"], writes=[PS(3)])
                for j in range(2):
                    P.dve(lambda e, j=j: e.scalar_tensor_tensor(out=RSf[:, j, :], in0=RSf[:, j, :], scalar=ret_chunk[:, j:j + 1],
                                                                in1=ps[2][:, j * 128:(j + 1) * 128], op0=ALU.mult, op1=ALU.add),
                          reads=["RSf", "ret_chunk", PS(2)], writes=["RSf"])
                P.act(lambda e: e.activation(out=RSb[:], in_=RSf[:], func=AF.Copy), reads=["RSf"], writes=["RSb"])
                P.act(lambda e: e.activation(out=sq[:], in_=ps[3][:, 0:256], func=AF.Square), reads=[PS(3)], writes=["rsq"])
                P.dve(lambda e: e.tensor_reduce(out=ss[:], in_=sq[:, :].rearrange("p (h v) -> p h v", h=4), axis=AX.X, op=ALU.add),
                      reads=["rsq"], writes=["rss"])
                P.dve(lambda e: e.tensor_scalar(out=rstd[:], in0=ss[:], scalar1=1.0 / 64, scalar2=HN_EPS, op0=ALU.mult, op1=ALU.add),
                      reads=["rss"], writes=["rrstd"])
                P.act(lambda e: e.activation(out=rstd[:], in_=rstd[:], func=AF.Sqrt), reads=["rrstd"], writes=["rrstd"])
                P.dve(lambda e: e.reciprocal(out=rstd[:], in_=rstd[:]), reads=["rrstd"], writes=["rrstd"])
                P.dve(lambda e: e.tensor_tensor(out=on[:], in0=ps[3][:, 0:256].rearrange("p (h v) -> p h v", h=4),
                                                in1=rstd[:, :].unsqueeze(2).to_broadcast([128, 4, 64]), op=ALU.mult),
                      reads=[PS(3), "rrstd"], writes=["ron"])
                P.pool(lambda e, c=c: e.tensor_tensor(out=mixr[:, c, :], in0=on[:, :, :].rearrange("p h v -> p (h v)"), in1=rsil[:, c, :], op=ALU.mult),
                       reads=["ron", "rsil"], writes=["mixr"])
            P.dma("sp", lambda e: e.dma_start(out=mixed[:, 768:1024].rearrange("(t p) c -> p t c", p=128), in_=mixr[:]),
                  reads=["mixr"], writes=["mixed_r"])
        _phase3()
        if stop == ("ret", l):
            return True

        def _phase4():
            phase_begin()
            dqh = [sb(f"dqh{i}", [128, S], BF16) for i in range(2)]
            dqm = [sb(f"dqm{i}", [128, 2, S], BF16) for i in range(2)]
            dkh = [sb(f"dkh{i}", [128, S], BF16) for i in range(2)]
            V1 = [sb(f"V1{i}", [128, NT, 129], BF16) for i in range(2)]
            mixd = sb("mixd", [128, NT, 512], BF16)
            pT = [[sb(f"pT{a}{m}", [128, SC], BF16) for m in range(2)] for a in range(2)]
            dl = sb("dl", [1, 4, 64], F32)
            prod = sb("prod", [1, 2, 64], F32)
            s2 = sb("s2", [1, 2], F32)
            e2 = sb("e2", [1, 2], F32)
            nlam1 = sb("nlam1", [128, 1], F32)
            nlam = sb("nlam", [128, 1], F32)
            dngs = sb("dngs", [128, 128], F32)
            rc = sb("rc", [128, 8], F32)
            a1 = sb("a1", [128, 128], F32)
            dh = sb("dh", [128, 4, 128], F32)
            dsq = sb("dsq", [128, 4, 128], F32)
            dss = sb("dss", [128, 4], F32)
            drs = sb("drs", [128, 4], F32)
            dn = sb("dn", [128, 4, 128], F32)
            for i in range(2):
                P.dve(lambda e, i=i: e.memset(V1[i][:], 1.0), writes=[f"V1{i}"])
            P.dma("sp", lambda e: e.dma_start(out=dl[:], in_=dlam_in[l:l + 1, :, :]), writes=["dl"])
            P.dma("sp", lambda e: e.dma_start(out=dngs[:], in_=dng_in[l:l + 1, :].partition_broadcast(128)), writes=["dngs"])
            P.dve(lambda e: e.tensor_scalar(out=dngs[:], in0=dngs[:], scalar1=1.0 - lam_init, scalar2=None, op0=ALU.mult), reads=["dngs"], writes=["dngs"])
            P.dve(lambda e: e.tensor_tensor(out=prod[:], in0=dl[:, 0:4:2, :], in1=dl[:, 1:4:2, :], op=ALU.mult), reads=["dl"], writes=["prod"])
            P.dve(lambda e: e.tensor_reduce(out=s2[:], in_=prod[:], axis=AX.X, op=ALU.add), reads=["prod"], writes=["s2"])
            P.act(lambda e: e.activation(out=e2[:], in_=s2[:], func=AF.Exp), reads=["s2"], writes=["e2"])
            P.dve(lambda e: e.memset(nlam1[:], 0.0), writes=["nlam1"])
            P.dve(lambda e: e.tensor_tensor(out=nlam1[0:1, :], in0=e2[:, 1:2], in1=e2[:, 0:1], op=ALU.subtract), reads=["e2"], writes=["nlam1"])
            P.dve(lambda e: e.tensor_scalar(out=nlam1[0:1, :], in0=nlam1[0:1, :], scalar1=-lam_init, scalar2=None, op0=ALU.add), reads=["nlam1"], writes=["nlam1"])
            P.pe(lambda e: e.matmul(ps[7][:, 0:1], lhsT=ones_f[:, :], rhs=nlam1[:, 0:1], start=True, stop=True), reads=["nlam1", "ones"], writes=[PS(7)])
            P.dve(lambda e: e.tensor_copy(out=nlam[:], in_=ps[7][:, 0:1]), reads=[PS(7)], writes=["nlam"])

            def acc_ap(m, sub):
                idx = m * 4 + sub
                return ps[4 + idx // 3], 4 + idx // 3, (idx % 3) * 129

            cnt = 0
            QT = S // SC
            SUBS = SC // 128
            for h in range(4):
                hi = h % 2
                P.dma("sp", lambda e, h=h, hi=hi: e.dma_start(out=dqh[hi][:], in_=zfm_b[h * 128:(h + 1) * 128, :]), writes=[f"dqh{hi}"])
                P.dma("sp", lambda e, h=h, hi=hi: e.dma_start(out=dkh[hi][:], in_=zfm_b[512 + h * 128:512 + (h + 1) * 128, :]), writes=[f"dk{hi}"])
                P.dma("sp", lambda e, h=h, hi=hi: e.dma_start(out=V1[hi][:, :, 0:128],
                                                              in_=ztm_b[:, 256 + h * 128:256 + (h + 1) * 128].rearrange("(t p) c -> p t c", p=128)),
                      writes=[f"V1{hi}"])
                for m in range(2):
                    P.dve(lambda e, hi=hi, m=m: e.tensor_scalar(out=dqm[hi][:, m, :], in0=dqh[hi][:], scalar1=halfmask[:, m:m + 1], scalar2=None, op0=ALU.mult),
                          reads=[f"dqh{hi}", "halfmask"], writes=[f"dq{hi}"])
                for jq in range(QT):
                    for b in (4, 5, 6):
                        P.dve(lambda e, b=b: e.memset(ps[b][:, :], 0.0), writes=[PS(b)])
                    nkb = SUBS * jq + SUBS
                    for kb in range(nkb):
                        i = kb - SUBS * jq
                        qlo = max(0, 128 * i)
                        n = SC - qlo
                        a = cnt % 2
                        cnt += 1
                        ksl = slice(kb * 128, (kb + 1) * 128)
                        for m in range(2):
                            bank = 2 * a + m
                            pr = slice(64 * m, 64 * m + 64)
                            near = i >= -1
                            P.pe(lambda e, bank=bank, m=m, hi=hi, ksl=ksl, qlo=qlo, jq=jq, near=near: e.matmul(
                                ps[bank][:, qlo:SC], lhsT=dkh[hi][:, ksl], rhs=dqm[hi][:, m, jq * SC + qlo:(jq + 1) * SC],
                                start=True, stop=not near), reads=[f"dq{hi}", f"dk{hi}"], writes=[PS(bank)])
                            if near:
                                c0 = 384 - 128 * i + qlo
                                P.pe(lambda e, bank=bank, h=h, qlo=qlo, c0=c0, n=n: e.matmul(
                                    ps[bank][:, qlo:SC], lhsT=anti_b[:], rhs=BTb[:, h, c0:c0 + n], start=False, stop=True),
                                    reads=["anti_b", "BTb"], writes=[PS(bank)])
                                P.act(lambda e, a=a, m=m, bank=bank, qlo=qlo: e.activation(out=pT[a][m][:, qlo:SC], in_=ps[bank][:, qlo:SC], func=AF.Exp),
                                      reads=[PS(bank)], writes=[f"pT{a}{m}"])
                            else:
                                P.act(lambda e, a=a, m=m, bank=bank, h=h: e.activation(out=pT[a][m][:, :], in_=ps[bank][:, 0:SC], func=AF.Exp,
                                                                                     bias=cfar[:, h:h + 1]),
                                      reads=[PS(bank), "cfar"], writes=[f"pT{a}{m}"])
                            for sub in range(qlo // 128, SUBS):
                                pst, bi, off = acc_ap(m, sub)
                                P.pe(lambda e, pst=pst, off=off, a=a, m=m, sub=sub, kb=kb, hi=hi: e.matmul(
                                    pst[:, off:off + 129], lhsT=pT[a][m][:, sub * 128:(sub + 1) * 128], rhs=V1[hi][:, kb, :],
                                    start=False, stop=False, skip_group_check=True),
                                    reads=[f"pT{a}{m}", f"V1{hi}"], writes=[PS(bi)])
                    for m in range(2):
                        for sub in range(SUBS):
                            pst, bi, off = acc_ap(m, sub)
                            P.dve(lambda e, pst=pst, off=off, m=m, sub=sub: e.reciprocal(out=rc[:, m * 4 + sub:m * 4 + sub + 1], in_=pst[:, off + 128:off + 129]),
                                  reads=[PS(bi)], writes=["rc"])
                    P.dve(lambda e: e.tensor_scalar(out=rc[:, 4:8], in0=rc[:, 4:8], scalar1=nlam[:, 0:1], scalar2=None, op0=ALU.mult),
                          reads=["rc", "nlam"], writes=["rc"])
                    for sub in range(SUBS):
                        p1, b1, o1 = acc_ap(0, sub)
                        p2, b2, o2 = acc_ap(1, sub)
                        P.act(lambda e, p1=p1, o1=o1, sub=sub: e.activation(out=a1[:], in_=p1[:, o1:o1 + 128], func=AF.Copy, scale=rc[:, sub:sub + 1]),
                              reads=[PS(b1), "rc"], writes=["a1"])
                        P.dve(lambda e, p2=p2, o2=o2, sub=sub: e.scalar_tensor_tensor(out=dh[:, sub, :], in0=p2[:, o2:o2 + 128], scalar=rc[:, 4 + sub:5 + sub],
                                                                                   in1=a1[:], op0=ALU.mult, op1=ALU.add),
                              reads=[PS(b2), "rc", "a1"], writes=["dh"])
                    P.act(lambda e: e.activation(out=dsq[:, 0:SUBS, :], in_=dh[:, 0:SUBS, :], func=AF.Square), reads=["dh"], writes=["dsq"])
                    P.dve(lambda e: e.tensor_reduce(out=dss[:, 0:SUBS], in_=dsq[:, 0:SUBS, :], axis=AX.X, op=ALU.add), reads=["dsq"], writes=["dss"])
                    P.dve(lambda e: e.tensor_scalar(out=drs[:, 0:SUBS], in0=dss[:, 0:SUBS], scalar1=1.0 / 128, scalar2=HN_EPS, op0=ALU.mult, op1=ALU.add),
                          reads=["dss"], writes=["drs"])
                    P.act(lambda e: e.activation(out=drs[:, 0:SUBS], in_=drs[:, 0:SUBS], func=AF.Sqrt), reads=["drs"], writes=["drs"])
                    P.dve(lambda e: e.reciprocal(out=drs[:, 0:SUBS], in_=drs[:, 0:SUBS]), reads=["drs"], writes=["drs"])
                    P.dve(lambda e: e.tensor_tensor(out=dn[:, 0:SUBS, :], in0=dh[:, 0:SUBS, :],
                                                    in1=drs[:, 0:SUBS].unsqueeze(2).to_broadcast([128, SUBS, 128]), op=ALU.mult),
                          reads=["dh", "drs"], writes=["dn"])
                    P.pool(lambda e, jq=jq, h=h: e.tensor_tensor(out=mixd[:, jq * SUBS:(jq + 1) * SUBS, h * 128:(h + 1) * 128], in0=dn[:, 0:SUBS, :],
                                                                 in1=dngs[:, :].unsqueeze(1).to_broadcast([128, SUBS, 128]), op=ALU.mult),
                           reads=["dn", "dngs"], writes=["mixd"])
            P.dma("sp", lambda e: e.dma_start(out=mixed[:, 256:768].rearrange("(t p) c -> p t c", p=128), in_=mixd[:]),
                  reads=["mixd"], writes=["mixed_d"])
        _phase4()
        if stop == ("diff", l):
            return True

        def _phase5():
            phase_begin()
            wout = sb("wout", [128, 8, D], BF16)
            wpg = sb("wpg", [128, 8, D], BF16)
            wpp = sb("wpp", [128, 2, D], BF16)
            wr = sb("wr", [128, 8, NE], F32)
            l1g = sb("l1g", [128, D], F32)
            l1b = sb("l1b", [128, D], F32)
            bpg = sb("bpg", [128, D], BF16)
            br = sb("br", [128, NE], F32)
            Macc = sb("Macc", [128, NE], F32)
            mt = [sb(f"mt{i}", [128, D], BF16) for i in range(2)]
            xi = [sb(f"xi{i}", [128, D], F32) for i in range(2)]
            pi = [sb(f"pi{i}", [128, PLE], F32) for i in range(2)]
            mT = sb("mT", [128, 8, 128], BF16)
            y = sb("y", [128, D], F32)
            st6 = sb("st6", [128, 12], F32)
            mv = sb("mv", [128, 2], F32)
            lrs = sb("lrs", [128, 1], F32)
            xn = sb("xn", [128, D], F32)
            x1 = [sb(f"x1{i}", [128, D], F32) for i in range(2)]
            x1b = [sb(f"x1b{i}", [128, D], BF16) for i in range(2)]
            x1Tf = sb("x1Tf", [128, 8, 128], F32)
            x1Tb = sb("x1Tb", [128, 8, 128], BF16)
            pTt = sb("pTt", [128, 2, 128], BF16)
            lg = sb("lg", [128, NE], F32)
            mx8 = sb("mx8", [128, 8], F32)
            Mm = sb("Mm", [128, NE], F32)
            nmx = sb("nmx", [128, 1], F32)
            ex = sb("ex", [128, NE], F32)
            den = sb("den", [128, 1], F32)
            Gt = sb("Gt", [128, NE], F32)
            kp1 = sb("kp1", [128, NE], F32)
            v01 = sb("v01", [128, NE], F32)
            key = sb("key", [128, NE], F32)
            t8 = sb("t8", [128, 8], F32)
            eq4 = sb("eq4", [128, 4, NE], F32)
            z4 = sb("z4", [128, 4], F32)
            posf = sb("posf", [128, 4], F32)
            posi = [sb(f"posi{i}", [128, 4], I32) for i in range(2)]
            sg = sb("sg", [128, D], F32)
            ee = sb("ee", [128, D], F32)
            r2t = [sb(f"r2t{i}", [128, D], F32) for i in range(2)]
            P.dma("pool", lambda e: e.dma_start(out=wout[:], in_=w_out[l].rearrange("(k p) n -> p k n", p=128)), writes=["wout"])
            P.dma("pool", lambda e: e.dma_start(out=wpg[:], in_=w_pg[l].rearrange("(k p) n -> p k n", p=128)), writes=["wpg"])
            P.dma("pool", lambda e: e.dma_start(out=wpp[:], in_=w_pp[l].rearrange("(k p) n -> p k n", p=128)), writes=["wpp"])
            P.dve(lambda e: e.memset(bpg[:], 0.0), writes=["bpg"])
            P.dve(lambda e: e.memset(br[:], 0.0), writes=["br"])
            P.dma("pool", lambda e: e.dma_start(out=bpg[0:1, :], in_=b_pg[l:l + 1, :]), writes=["bpg"])
            P.dma("sp", lambda e: e.dma_start(out=wr[:], in_=w_router[l].rearrange("(k p) n -> p k n", p=128)), writes=["wr"])
            P.dma("sp", lambda e: e.dma_start(out=br[0:1, :], in_=b_router[l:l + 1, :]), writes=["br"])
            P.dma("sp", lambda e: e.dma_start(out=l1g[:], in_=ln1g[l:l + 1, :].partition_broadcast(128)), writes=["lng"])
            P.dma("sp", lambda e: e.dma_start(out=l1b[:], in_=ln1b[l:l + 1, :].partition_broadcast(128)), writes=["lnb"])
            P.dma("sp", lambda e: e.dma_start(out=lst, in_=cin["list_init"]), writes=["lst"])
            P.dve(lambda e: e.memset(Macc[:], 0.0), writes=["Macc"])

            for t in range(NT):
                tsl = slice(t * 128, (t + 1) * 128)
                ti = t % 2
                P.dma("sp", lambda e, ti=ti, tsl=tsl: e.dma_start(out=mt[ti][:], in_=mixed[tsl, :]), writes=[f"mt{ti}"])
                P.dma("sp", lambda e, ti=ti, tsl=tsl: e.dma_start(out=xi[ti][:], in_=X_in[tsl, :]), writes=[f"xi{ti}"])
                P.dma("sp", lambda e, ti=ti, tsl=tsl: e.dma_start(out=pi[ti][:], in_=p_in[l, tsl, :]), writes=[f"pi{ti}"])
                for hb in range(2):
                    for kk in range(4):
                        k = hb * 4 + kk
                        P.pe(lambda e, hb=hb, kk=kk, k=k, ti=ti: e.matmul(ps[hb][:, kk * 128:(kk + 1) * 128], lhsT=mt[ti][:, k * 128:(k + 1) * 128],
                                                                         rhs=ident_b[:], start=True, stop=True),
                             reads=[f"mt{ti}", "ident_b"], writes=[PS(hb)])
                    if hb == 0:
                        P.act(lambda e, hb=hb: e.activation(out=mT[:, 0:4, :], in_=ps[0][:, :].rearrange("p (k n) -> p k n", k=4), func=AF.Copy),
                              reads=[PS(0)], writes=["mT"])
                    else:
                        P.dve(lambda e, hb=hb: e.tensor_copy(out=mT[:, 4:8, :], in_=ps[1][:, :].rearrange("p (k n) -> p k n", k=4)),
                              reads=[PS(1)], writes=["mT"])
                for hb in range(2):
                    for k in range(8):
                        P.pe(lambda e, hb=hb, k=k: e.matmul(ps[2 + hb][:, :], lhsT=mT[:, k, :], rhs=wout[:, k, hb * 512:(hb + 1) * 512],
                                                           start=(k == 0), stop=(k == 7)), reads=["mT", "wout"], writes=[PS(2 + hb)])
                    P.dve(lambda e, hb=hb, ti=ti: e.scalar_tensor_tensor(out=y[:, hb * 512:(hb + 1) * 512], in0=xi[ti][:, hb * 512:(hb + 1) * 512],
                                                                         scalar=ALPHA, in1=ps[2 + hb][:, :], op0=ALU.mult, op1=ALU.add),
                          reads=[f"xi{ti}", PS(2 + hb)], writes=["y"])
                layer_norm(y, x1[ti], l1g, l1b, ("y", f"x1{ti}"), st6, mv, lrs, xn)
                P.act(lambda e, ti=ti: e.activation(out=x1b[ti][:], in_=x1[ti][:], func=AF.Copy), reads=[f"x1{ti}"], writes=[f"x1b{ti}"])
                P.dma("sp", lambda e, ti=ti, tsl=tsl: e.dma_start(out=h_bf[tsl, :], in_=x1b[ti][:]), reads=[f"x1b{ti}"], writes=[f"h_bf{t}"])
                for hb in range(2):
                    for kk in range(4):
                        k = hb * 4 + kk
                        P.pe(lambda e, hb=hb, kk=kk, k=k, ti=ti: e.matmul(ps[4 + hb][:, kk * 128:(kk + 1) * 128], lhsT=x1[ti][:, k * 128:(k + 1) * 128],
                                                                         rhs=ident_f[:], start=True, stop=True),
                             reads=[f"x1{ti}", "ident"], writes=[PS(4 + hb)])
                    P.act(lambda e, hb=hb: e.activation(out=x1Tf[:, hb * 4:hb * 4 + 4, :], in_=ps[4 + hb][:, :].rearrange("p (k n) -> p k n", k=4), func=AF.Copy),
                          reads=[PS(4 + hb)], writes=["x1Tf"])
                    P.dve(lambda e, hb=hb: e.tensor_copy(out=x1Tb[:, hb * 4:hb * 4 + 4, :], in_=ps[4 + hb][:, :].rearrange("p (k n) -> p k n", k=4)),
                          reads=[PS(4 + hb)], writes=["x1Tb"])
                for k in range(8):
                    P.pe(lambda e, k=k: e.matmul(ps[6][:, 0:NE], lhsT=x1Tf[:, k, :], rhs=wr[:, k, :], start=(k == 0), stop=False),
                         reads=["x1Tf", "wr"], writes=[PS(6)])
                P.pe(lambda e: e.matmul(ps[6][:, 0:NE], lhsT=row0_f[:, :], rhs=br[:, :], start=False, stop=True),
                     reads=["row0", "br"], writes=[PS(6)])
                P.dve(lambda e: e.tensor_copy(out=lg[:], in_=ps[6][:, 0:NE]), reads=[PS(6)], writes=["lg"])
                P.dve(lambda e: e.max(out=mx8[:], in_=lg[:]), reads=["lg"], writes=["mx8"])
                P.dve(lambda e: e.tensor_scalar(out=Mm[:], in0=lg[:], scalar1=mx8[:, 3:4], scalar2=None, op0=ALU.is_ge), reads=["lg", "mx8"], writes=["Mm"])
                P.dve(lambda e: e.tensor_scalar(out=nmx[:], in0=mx8[:, 0:1], scalar1=-1.0, scalar2=None, op0=ALU.mult), reads=["mx8"], writes=["nmx"])
                P.act(lambda e: e.activation(out=ex[:], in_=lg[:], func=AF.Exp, bias=nmx[:, 0:1]), reads=["lg", "nmx"], writes=["ex"])
                P.dve(lambda e: e.tensor_tensor(out=ex[:], in0=ex[:], in1=Mm[:], op=ALU.mult), reads=["ex", "Mm"], writes=["ex"])
                P.dve(lambda e: e.tensor_reduce(out=den[:], in_=ex[:], axis=AX.X, op=ALU.add), reads=["ex"], writes=["den"])
                P.dve(lambda e: e.reciprocal(out=den[:], in_=den[:]), reads=["den"], writes=["den"])
                P.pe(lambda e: e.matmul(ps[6][:, 64:64 + NE], lhsT=lstrict[:], rhs=Mm[:], start=True, stop=False), reads=["lstrict", "Mm"], writes=[PS(6)])
                P.pe(lambda e: e.matmul(ps[6][:, 64:64 + NE], lhsT=ones_f[:], rhs=Macc[:], start=False, stop=True), reads=["ones", "Macc"], writes=[PS(6)])
                P.dve(lambda e: e.tensor_tensor(out=kp1[:], in0=ps[6][:, 64:64 + NE], in1=ecap[:], op=ALU.add), reads=[PS(6), "ecap"], writes=["kp1"])
                P.dve(lambda e: e.tensor_scalar(out=v01[:], in0=ps[6][:, 64:64 + NE], scalar1=float(C) - 0.5, scalar2=None, op0=ALU.is_lt), reads=[PS(6)], writes=["v01"])
                P.dve(lambda e: e.tensor_tensor(out=Macc[:], in0=Macc[:], in1=Mm[:], op=ALU.add), reads=["Macc", "Mm"], writes=["Macc"])
                P.dve(lambda e: e.tensor_tensor(out=v01[:], in0=v01[:], in1=Mm[:], op=ALU.mult), reads=["v01", "Mm"], writes=["v01"])
                P.dve(lambda e: e.tensor_tensor(out=key[:], in0=kp1[:], in1=v01[:], op=ALU.mult), reads=["kp1", "v01"], writes=["key"])
                P.dve(lambda e: e.scalar_tensor_tensor(out=Gt[:], in0=ex[:], scalar=den[:, 0:1], in1=v01[:], op0=ALU.mult, op1=ALU.mult),
                      reads=["ex", "den", "v01"], writes=["Gt"])
                P.dve(lambda e: e.max(out=t8[:], in_=key[:]), reads=["key"], writes=["t8"])
                for j in range(4):
                    P.dve(lambda e, j=j: e.tensor_scalar(out=eq4[:, j, :], in0=key[:], scalar1=t8[:, j:j + 1], scalar2=None, op0=ALU.is_equal),
                          reads=["key", "t8"], writes=["eq4"])
                P.dve(lambda e: e.tensor_tensor(out=eq4[:], in0=eq4[:], in1=Gt[:, :].unsqueeze(1).to_broadcast([128, 4, NE]), op=ALU.mult),
                      reads=["eq4", "Gt"], writes=["eq4"])
                P.dve(lambda e, t=t: e.tensor_reduce(out=g4all[:, t, :], in_=eq4[:], axis=AX.X, op=ALU.add), reads=["eq4"], writes=["g4all"])
                P.dve(lambda e: e.tensor_scalar(out=z4[:], in0=t8[:, 0:4], scalar1=0.5, scalar2=BIGPOS, op0=ALU.is_lt, op1=ALU.mult), reads=["t8"], writes=["z4"])
                P.dve(lambda e: e.scalar_tensor_tensor(out=posf[:], in0=t8[:, 0:4], scalar=-1.0, in1=z4[:], op0=ALU.add, op1=ALU.add),
                      reads=["t8", "z4"], writes=["posf"])
                P.dve(lambda e, ti=ti: e.tensor_copy(out=posi[ti][:], in_=posf[:]), reads=["posf"], writes=[f"posi{ti}"])
                for j in range(4):
                    P.dma("pool", lambda e, j=j, t=t, ti=ti: e.indirect_dma_start(
                        out=lst, out_offset=bass.IndirectOffsetOnAxis(ap=posi[ti][:, j:j + 1], axis=0),
                        in_=tokslot[:, (t * 4 + j) * 2:(t * 4 + j) * 2 + 2], in_offset=None,
                        bounds_check=P.reg(e, NE * C - 1), oob_is_err=False), reads=[f"posi{ti}", "tokslot", "lst"], writes=[f"lst_w{t}_{j}"])
                for kk in range(2):
                    P.pe(lambda e, kk=kk, ti=ti: e.matmul(ps[7][:, kk * 128:(kk + 1) * 128], lhsT=pi[ti][:, kk * 128:(kk + 1) * 128], rhs=ident_f[:],
                                                         start=True, stop=True), reads=[f"pi{ti}", "ident"], writes=[PS(7)])
                P.act(lambda e: e.activation(out=pTt[:], in_=ps[7][:, 0:256].rearrange("p (k n) -> p k n", k=2), func=AF.Copy), reads=[PS(7)], writes=["pTt"])
                for hb in range(2):
                    for k in range(8):
                        P.pe(lambda e, hb=hb, k=k: e.matmul(ps[hb][:, :], lhsT=x1Tb[:, k, :], rhs=wpg[:, k, hb * 512:(hb + 1) * 512],
                                                           start=(k == 0), stop=False), reads=["x1Tb", "wpg"], writes=[PS(hb)])
                    P.pe(lambda e, hb=hb: e.matmul(ps[hb][:, :], lhsT=row0_b[:, :], rhs=bpg[:, hb * 512:(hb + 1) * 512], start=False, stop=True),
                         reads=["row0_b", "bpg"], writes=[PS(hb)])
                    P.act(lambda e, hb=hb: e.activation(out=sg[:, hb * 512:(hb + 1) * 512], in_=ps[hb][:, :], func=AF.Sigmoid), reads=[PS(hb)], writes=["sg"])
                    for kk in range(2):
                        P.pe(lambda e, hb=hb, kk=kk: e.matmul(ps[2 + hb][:, :], lhsT=pTt[:, kk, :], rhs=wpp[:, kk, hb * 512:(hb + 1) * 512],
                                                             start=(kk == 0), stop=(kk == 1)), reads=["pTt", "wpp"], writes=[PS(2 + hb)])
                    P.dve(lambda e, hb=hb: e.tensor_tensor(out=ee[:, hb * 512:(hb + 1) * 512], in0=sg[:, hb * 512:(hb + 1) * 512], in1=ps[2 + hb][:, :], op=ALU.mult),
                          reads=["sg", PS(2 + hb)], writes=["ee"])
                P.dve(lambda e, ti=ti: e.scalar_tensor_tensor(out=r2t[ti][:], in0=x1[ti][:], scalar=ALPHA, in1=ee[:], op0=ALU.mult, op1=ALU.add),
                       reads=[f"x1{ti}", "ee"], writes=[f"r2t{ti}"])
                P.dma("sp", lambda e, ti=ti, tsl=tsl: e.dma_start(out=r2[tsl, :], in_=r2t[ti][:]), reads=[f"r2t{ti}"], writes=[f"r2_{t}"])
        _phase5()
        if stop == ("c", l):
            return True

        def _phase6():
            phase_begin()
            wgu = [sb(f"wgu{i}", [128, 8, 2 * D], BF16) for i in range(2)]
            wdn = [sb(f"wdn{i}", [128, 8, D], BF16) for i in range(2)]
            bgu = sb("bgu", [128, NE, 16], F32)
            bdn = [sb(f"bdn{i}", [128, D], BF16) for i in range(2)]
            ltl = [sb(f"ltl{i}", [128, NR, 2], I32) for i in range(2)]
            hg = [sb(f"hg{i}", [128, D], BF16) for i in range(2)]
            hT = sb("hT", [128, 8, C], BF16)
            actT = sb("actT", [128, 8, C], BF16)
            g1 = [sb(f"g1{i}", [128, 512], F32) for i in range(2)]
            sgm = [sb(f"sgm{i}", [128, 512], F32) for i in range(2)]
            u1 = [sb(f"u1{i}", [128, 512], F32) for i in range(2)]
            yo = [sb(f"yo{i}", [128, D], F32) for i in range(2)]
            P.dma("sp", lambda e: e.dma_start(out=bgu[:], in_=b_gu[l]), writes=["bgu"])
            for i in range(2):
                P.dve(lambda e, i=i: e.memset(bdn[i][:], 0.0), writes=[f"bdn{i}"])
            segs = []
            lo = 0
            while lo < C:
                n = min(384 if C % 384 == 0 else 512, C - lo)
                segs.append((lo, n))
                lo += n
            ci = 0
            for ex_ in range(NE):
                ei = ex_ % 2
                for q in range(4):
                    P.dma("pool", lambda e, q=q, ei=ei, ex_=ex_: e.dma_start(
                        out=wgu[ei][:, 2 * q:2 * q + 2, :], in_=w_gu[l, ex_, q * 256:(q + 1) * 256, :].rearrange("(k p) n -> p k n", p=128)),
                        writes=[f"wgu{ei}_{q}"])
                for q in range(2):
                    P.dma("pool", lambda e, q=q, ei=ei, ex_=ex_: e.dma_start(
                        out=wdn[ei][:, 4 * q:4 * q + 4, :], in_=w_down[l, ex_, q * 512:(q + 1) * 512, :].rearrange("(k p) n -> p k n", p=128)),
                        writes=[f"wdn{ei}_{q}"])
                P.dma("pool", lambda e, ei=ei, ex_=ex_: e.dma_start(out=bdn[ei][0:1, :], in_=b_down[l, ex_:ex_ + 1, :]), writes=[f"bdn{ei}"])
                P.dma("sp", lambda e, ei=ei, ex_=ex_: e.dma_start(out=ltl[ei][:], in_=lst[ex_ * C:(ex_ + 1) * C, :].rearrange("(r p) c -> p r c", p=128)),
                      reads=["lst"], writes=[f"ltl{ei}"])
                WGU = [f"wgu{ei}_{q}" for q in range(4)]
                WDN = [f"wdn{ei}_{q}" for q in range(2)]
                for r in range(NR):
                    gi = r % 2
                    P.dma("pool", lambda e, gi=gi, ei=ei, r=r: e.indirect_dma_start(
                        out=hg[gi][:], out_offset=None, in_=h_bf, in_offset=bass.IndirectOffsetOnAxis(ap=ltl[ei][:, r, 0:1], axis=0),
                        bounds_check=P.reg(e, S), oob_is_err=False), reads=[f"ltl{ei}", "h_bf"], writes=[f"hg{gi}"])
                    for hb in range(2):
                        for kk in range(4):
                            k = hb * 4 + kk
                            P.pe(lambda e, hb=hb, kk=kk, k=k, gi=gi: e.matmul(ps[hb][:, kk * 128:(kk + 1) * 128], lhsT=hg[gi][:, k * 128:(k + 1) * 128],
                                                                             rhs=ident_b[:], start=True, stop=True),
                                 reads=[f"hg{gi}", "ident_b"], writes=[PS(hb)])
                        if hb == 0:
                            P.act(lambda e, r=r: e.activation(out=hT[:, 0:4, r * 128:(r + 1) * 128], in_=ps[0][:, :].rearrange("p (k n) -> p k n", k=4), func=AF.Copy),
                                  reads=[PS(0)], writes=["hT"])
                        else:
                            P.dve(lambda e, r=r: e.tensor_copy(out=hT[:, 4:8, r * 128:(r + 1) * 128], in_=ps[1][:, :].rearrange("p (k n) -> p k n", k=4)),
                                  reads=[PS(1)], writes=["hT"])
                for jf in range(8):
                    for (lo, n) in segs:
                        a = ci % 2
                        ci += 1
                        bg_, bu_ = 2 + 2 * a, 3 + 2 * a
                        for k in range(8):
                            P.pe(lambda e, k=k, jf=jf, lo=lo, n=n, bg_=bg_, ei=ei: e.matmul(ps[bg_][:, 0:n], lhsT=wgu[ei][:, k, jf * 128:(jf + 1) * 128],
                                                                                       rhs=hT[:, k, lo:lo + n], start=(k == 0), stop=(k == 7)),
                                 reads=["hT", f"wgu{ei}_{k // 2}"], writes=[PS(bg_)])
                        for k in range(8):
                            P.pe(lambda e, k=k, jf=jf, lo=lo, n=n, bu_=bu_, ei=ei: e.matmul(ps[bu_][:, 0:n], lhsT=wgu[ei][:, k, D + jf * 128:D + (jf + 1) * 128],
                                                                                       rhs=hT[:, k, lo:lo + n], start=(k == 0), stop=(k == 7)),
                                 reads=["hT", f"wgu{ei}_{k // 2}"], writes=[PS(bu_)])
                        P.dve(lambda e, a=a, bg_=bg_, n=n, ex_=ex_, jf=jf: e.tensor_scalar(out=g1[a][:, 0:n], in0=ps[bg_][:, 0:n], scalar1=bgu[:, ex_, jf:jf + 1],
                                                                                        scalar2=7.0, op0=ALU.add, op1=ALU.min),
                              reads=[PS(bg_), "bgu"], writes=[f"g1{a}"])
                        P.act(lambda e, a=a, n=n: e.activation(out=sgm[a][:, 0:n], in_=g1[a][:, 0:n], func=AF.Sigmoid, scale=1.702),
                              reads=[f"g1{a}"], writes=[f"sgm{a}"])
                        P.dve(lambda e, a=a, bu_=bu_, n=n, ex_=ex_, jf=jf: e.tensor_scalar(out=u1[a][:, 0:n], in0=ps[bu_][:, 0:n], scalar1=bgu[:, ex_, 8 + jf:9 + jf],
                                                                                        scalar2=7.0, op0=ALU.add, op1=ALU.min),
                              reads=[PS(bu_), "bgu"], writes=[f"u1{a}"])
                        P.dve(lambda e, a=a, n=n: e.tensor_scalar(out=u1[a][:, 0:n], in0=u1[a][:, 0:n], scalar1=-7.0, scalar2=1.0, op0=ALU.max, op1=ALU.add),
                               reads=[f"u1{a}"], writes=[f"u1{a}"])
                        P.pool(lambda e, a=a, n=n: e.tensor_tensor(out=g1[a][:, 0:n], in0=g1[a][:, 0:n], in1=sgm[a][:, 0:n], op=ALU.mult),
                               reads=[f"g1{a}", f"sgm{a}"], writes=[f"g1{a}"])
                        P.dve(lambda e, a=a, n=n, jf=jf, lo=lo: e.tensor_tensor(out=actT[:, jf, lo:lo + n], in0=g1[a][:, 0:n], in1=u1[a][:, 0:n], op=ALU.mult),
                              reads=[f"g1{a}", f"u1{a}"], writes=["actT"])
                for r in range(NR):
                    yi = r % 2
                    for hb in range(2):
                        bank = 6 + hb
                        for k in range(8):
                            P.pe(lambda e, k=k, r=r, hb=hb, bank=bank, ei=ei: e.matmul(ps[bank][:, :], lhsT=actT[:, k, r * 128:(r + 1) * 128],
                                                                                     rhs=wdn[ei][:, k, hb * 512:(hb + 1) * 512], start=(k == 0), stop=False),
                                 reads=["actT", f"wdn{ei}_{k // 4}"], writes=[PS(bank)])
                        P.pe(lambda e, hb=hb, bank=bank, ei=ei: e.matmul(ps[bank][:, :], lhsT=row0_b[:, :], rhs=bdn[ei][:, hb * 512:(hb + 1) * 512],
                                                                        start=False, stop=True), reads=["row0_b", f"bdn{ei}"], writes=[PS(bank)])
                        if hb == 0:
                            P.act(lambda e, yi=yi, bank=bank: e.activation(out=yo[yi][:, 0:512], in_=ps[bank][:, :], func=AF.Copy), reads=[PS(bank)], writes=[f"yo{yi}"])
                        else:
                            P.dve(lambda e, yi=yi, bank=bank: e.tensor_copy(out=yo[yi][:, 512:1024], in_=ps[bank][:, :]), reads=[PS(bank)], writes=[f"yo{yi}"])
                    P.dma("pool", lambda e, yi=yi, ei=ei, r=r: e.indirect_dma_start(
                        out=ybuf, out_offset=bass.IndirectOffsetOnAxis(ap=ltl[ei][:, r, 1:2], axis=0), in_=yo[yi][:], in_offset=None,
                        bounds_check=P.reg(e, 4 * S - 1), oob_is_err=False), reads=[f"yo{yi}", f"ltl{ei}"], writes=[f"ybuf_{ex_}_{r}"])
        _phase6()
        if stop == ("d", l):
            return True

        def _phase7():
            phase_begin()
            l2g = sb("l2g", [128, D], F32)
            l2b = sb("l2b", [128, D], F32)
            rr = [sb(f"rr{i}", [128, D], F32) for i in range(2)]
            yb = [sb(f"yb{i}", [128, 4, D], F32) for i in range(2)]
            xo = [sb(f"xo{i}", [128, D], F32) for i in range(2)]
            st6 = sb("st6e", [128, 12], F32)
            mv = sb("mve", [128, 2], F32)
            lrs = sb("lrse", [128, 1], F32)
            xn = sb("xne", [128, D], F32)
            P.dma("sp", lambda e: e.dma_start(out=l2g[:], in_=ln2g[l:l + 1, :].partition_broadcast(128)), writes=["lng"])
            P.dma("sp", lambda e: e.dma_start(out=l2b[:], in_=ln2b[l:l + 1, :].partition_broadcast(128)), writes=["lnb"])
            ybv2 = ybuf.rearrange("(t p j) d -> t p j d", p=128, j=4)
            for t in range(NT):
                tsl = slice(t * 128, (t + 1) * 128)
                ti = t % 2
                P.dma("sp", lambda e, ti=ti, tsl=tsl: e.dma_start(out=rr[ti][:], in_=r2[tsl, :]), writes=[f"rr{ti}"])
                P.dma("pool", lambda e, ti=ti, t=t: e.dma_start(out=yb[ti][:], in_=ybv2[t]), writes=[f"yb{ti}"])
                for j in range(4):
                    eng = P.dve
                    eng(lambda e, ti=ti, j=j, t=t: e.scalar_tensor_tensor(out=rr[ti][:], in0=yb[ti][:, j, :], scalar=g4all[:, t, j:j + 1], in1=rr[ti][:],
                                                                          op0=ALU.mult, op1=ALU.add), reads=[f"yb{ti}", f"rr{ti}", "g4all"], writes=[f"rr{ti}"])
                layer_norm(rr[ti], xo[ti], l2g, l2b, (f"rr{ti}", f"xo{ti}"), st6, mv, lrs, xn)
                P.dma("sp", lambda e, ti=ti, tsl=tsl: e.dma_start(out=X_out[tsl, :], in_=xo[ti][:]), reads=[f"xo{ti}"], writes=[f"xout{t}"])
        _phase7()
        return False

    for l_ in range(L):
        if layer(l_):
            break
    P.emit(max_ops)
    return nc, P


def prep_shared(inp, S, C, L):
    perm = win_col_perm()
    sh = {}
    sh["w_in"] = np.ascontiguousarray(inp["w_in"][:L][:, :, perm])
    sh["wg_aug"] = np.ascontiguousarray(np.concatenate([inp["w_gla_gate"][:L], inp["b_gla_gate"][:L, None, :]], axis=1))
    sh["gla_norm_g"] = np.ascontiguousarray(inp["gla_norm_g"][:L])
    sh["diff_lambda"] = np.ascontiguousarray(inp["diff_lambda"][:L])
    sh["diff_norm_g"] = np.ascontiguousarray(inp["diff_norm_g"][:L])
    sh["w_out"] = np.ascontiguousarray(inp["w_out"][:L])
    sh["rb_aug"] = np.ascontiguousarray(np.concatenate([inp["rel_bias"], np.full((1, 4), NEG, np.float32)], axis=0))
    for k in ("ln1_g", "ln1_b", "w_router", "b_router", "w_down", "b_down", "ln2_g", "ln2_b"):
        sh[k] = np.ascontiguousarray(inp[k][:L])
    gu_perm = np.concatenate([np.arange(0, 2 * D, 2), np.arange(1, 2 * D, 2)])
    sh["w_gu"] = np.ascontiguousarray(inp["w_gate_up"][:L][:, :, :, gu_perm])
    bg = inp["b_gate_up"][:L][:, :, gu_perm]
    sh["b_gu"] = np.ascontiguousarray(bg.reshape(L, NE, 16, 128).transpose(0, 3, 1, 2))
    sh["w_pg"] = np.ascontiguousarray(inp["w_ple_gate"][:L])
    sh["b_pg"] = np.ascontiguousarray(inp["b_ple_gate"][:L])
    sh["w_pp"] = np.ascontiguousarray(inp["w_ple_proj"][:L])
    for k, v in host_consts(S, C).items():
        sh["c_" + k] = v
    return sh


def kernel(**inputs):
    inp = {k: np.asarray(v) for k, v in inputs.items()}
    B, S, _ = inp["x"].shape
    L = inp["w_in"].shape[0]
    C = 768
    nc, _ = build(S, L, C)
    sh = prep_shared(inp, S, C, L)
    in_maps = []
    for b in range(B):
        m = dict(sh)
        m["x"] = np.ascontiguousarray(inp["x"][b])
        m["p"] = np.ascontiguousarray(inp["p"][:, b])
        in_maps.append(m)
    res = run_bass_kernel_spmd(nc, in_maps, core_ids=list(range(B)))
    return np.stack([np.asarray(r["out"]) for r in res.results], axis=0).astype(np.float32)
```

```python
import math
import types
import numpy as np
import concourse.bass as bass
import concourse.mybir as mybir
from concourse.bass_utils import run_bass_kernel_spmd

F32 = mybir.dt.float32
BF16 = mybir.dt.bfloat16
I32 = mybir.dt.int32
ALU = mybir.AluOpType
AF = mybir.ActivationFunctionType
AX = mybir.AxisListType

D = 1024
NE = 32
PLE = 256
ALPHA = 8 ** 0.25
LN_EPS = 1e-5
HN_EPS = 1e-5
SEM_LIMIT = 12000
N_DMA_SEMS = 12
NEG = -30000.0
BIGPOS = 1.0e6


class Op:
    __slots__ = ("eng", "fn", "reads", "writes", "dma", "deps", "sig", "idx", "semref")

    def __init__(self, eng, fn, reads, writes, dma):
        self.eng = eng
        self.fn = fn
        self.reads = reads
        self.writes = writes
        self.dma = dma
        self.deps = []
        self.sig = False
        self.semref = None


def _freeze(fn):
    if fn is None or fn.__closure__ is None:
        return fn
    cells = []
    for c in fn.__closure__:
        try:
            cells.append(types.CellType(c.cell_contents))
        except ValueError:
            cells.append(c)
    return types.FunctionType(fn.__code__, fn.__globals__, fn.__name__, fn.__defaults__, tuple(cells))


class Prog:
    ENGS = ("pe", "dve", "act", "pool", "sp")

    def __init__(self, nc):
        self.nc = nc
        self.ops = []
        self.last_w = {}
        self.readers = {}
        self.since_bar = []

    def add(self, eng, fn, reads=(), writes=(), dma=False, extra_deps=()):
        reads = list(reads)
        writes = list(writes)
        for r in list(reads):
            if r.startswith("ps") and r[2:].isdigit():
                reads.remove(r)
                if r not in writes:
                    writes.append(r)
        fn = _freeze(fn)
        op = Op(eng, fn, tuple(reads), tuple(writes), dma)
        op.idx = len(self.ops)
        deps = set(extra_deps)
        for r in op.reads:
            w = self.last_w.get(r)
            if w is not None:
                deps.add(w)
        for w_ in op.writes:
            w = self.last_w.get(w_)
            if w is not None:
                deps.add(w)
            for rd in self.readers.get(w_, ()):
                deps.add(rd)
        deps.discard(op.idx)
        for r in op.reads:
            self.readers.setdefault(r, []).append(op.idx)
        for w_ in op.writes:
            self.last_w[w_] = op.idx
            self.readers[w_] = []
        op.deps = sorted(deps)
        self.ops.append(op)
        self.since_bar.append(op.idx)
        return op

    def pe(self, fn, reads=(), writes=()):
        return self.add("pe", fn, reads, writes)

    def dve(self, fn, reads=(), writes=()):
        return self.add("dve", fn, reads, writes)

    def act(self, fn, reads=(), writes=()):
        return self.add("act", fn, reads, writes)

    def pool(self, fn, reads=(), writes=()):
        return self.add("pool", fn, reads, writes)

    def dma(self, eng, fn, reads=(), writes=()):
        return self.add(eng, fn, reads, writes, dma=True)

    def reg(self, eng, value):
        if not hasattr(self, "_regs"):
            self._regs = {}
        if value not in self._regs:
            r = eng.alloc_register(f"c{value}")
            eng.reg_mov(r, value)
            self._regs[value] = r
        return self._regs[value]

    def barrier(self):
        prev = list(self.since_bar)
        last = {}
        dmas = []
        for i in prev:
            o = self.ops[i]
            if o.fn is None:
                continue
            if o.dma:
                dmas.append(i)
            else:
                last[o.eng] = i
        deps = dmas + list(last.values())
        self.since_bar = []
        for e in self.ENGS:
            self.add(e, None, extra_deps=deps)
        self.last_w = {}
        self.readers = {}

    def emit(self, max_ops=None):
        nc = self.nc
        if max_ops is not None:
            self.ops = self.ops[:max_ops]
        ops = self.ops

        def need_wait(dop, op):
            if dop.dma or op.dma:
                return True
            if dop.eng == "pe" and op.eng == "pe":
                return False
            return True

        for op in ops:
            if op.dma:
                op.sig = True
        for op in ops:
            for d in op.deps:
                if need_wait(ops[d], op):
                    ops[d].sig = True
        eng_sems = {e: [] for e in self.ENGS}
        eng_count = {e: 0 for e in self.ENGS}
        dma_sems = {e: [] for e in self.ENGS}
        dma_cnt = {}
        dma_rr = {e: 0 for e in self.ENGS}
        all_final = {}
        for op in ops:
            if not op.sig:
                continue
            if op.dma:
                lst = dma_sems[op.eng]
                k = dma_rr[op.eng] % N_DMA_SEMS
                dma_rr[op.eng] += 1
                if k >= len(lst):
                    lst.append(nc.alloc_semaphore(f"d_{op.eng}_{k}"))
                s = lst[k]
                c = dma_cnt.get(s.name, 0) + 1
                dma_cnt[s.name] = c
                op.semref = (s, 16 * c, 16)
            else:
                n = eng_count[op.eng]
                eng_count[op.eng] = n + 1
                ep = n // SEM_LIMIT
                lst = eng_sems[op.eng]
                if ep >= len(lst):
                    lst.append(nc.alloc_semaphore(f"e_{op.eng}_{ep}"))
                op.semref = (lst[ep], n % SEM_LIMIT + 1, 1)
            all_final[op.semref[0].name] = (op.semref[0], op.semref[1])
        per_eng = {e: [o for o in ops if o.eng == e] for e in self.ENGS}
        self.n_waits = 0

        def run_engine(ename, eng):
            seen = {}

            def wait(sem, val):
                if seen.get(sem.name, 0) >= val:
                    return
                seen[sem.name] = val
                eng.wait_ge(sem, val)
                self.n_waits += 1

            for op in per_eng[ename]:
                for d in op.deps:
                    dop = ops[d]
                    if dop.semref is None or not need_wait(dop, op):
                        continue
                    wait(dop.semref[0], dop.semref[1])
                if op.fn is None:
                    continue
                if op.dma:
                    s, v, inc = op.semref
                    if v > 16:
                        wait(s, v - 16)
                ins = op.fn(eng)
                if op.semref is not None:
                    ins.then_inc(op.semref[0], op.semref[2])
            if ename == "sp":
                for s, v in all_final.values():
                    wait(s, v)

        with nc.Block() as block:
            @block.tensor
            def _(e):
                run_engine("pe", e)

            @block.vector
            def _(e):
                run_engine("dve", e)

            @block.scalar
            def _(e):
                run_engine("act", e)

            @block.gpsimd
            def _(e):
                run_engine("pool", e)

            @block.sync
            def _(e):
                run_engine("sp", e)


_SPL = dict(gq=(0, 128), gk=(128, 256), gv=(256, 512), glr=(512, 528), gout=(528, 784),
            dq=(784, 1296), dk=(1296, 1808), dv=(1808, 2320), rq=(2320, 2576), rk=(2576, 2832),
            rv=(2832, 3088), rg=(3088, 3344))


def _swap_pairs(idx):
    idx = np.asarray(idx).reshape(-1, 2)
    return idx[:, ::-1].reshape(-1)


def win_col_perm():
    r = lambda k: np.arange(*_SPL[k])
    cols = []
    cols += [r("gq"), r("gk"), r("glr"), r("dq"), r("dk"), r("rq"), _swap_pairs(r("rq")),
             r("rk"), _swap_pairs(r("rk"))]
    cols += [r("gk"), r("gv"), r("gout"), r("rg"), r("dv"), r("rv")]
    return np.concatenate(cols)


FM_GQ, FM_GK, FM_GLR, FM_DQ, FM_DK, FM_RQ, FM_RQS, FM_RK, FM_RKS = 0, 128, 256, 272, 784, 1296, 1552, 1808, 2064
FM_END = 2320
TM0 = FM_END
TM1 = TM0 + 384
TM2 = TM1 + 512
TM3 = TM2 + 512
NCOL = TM3 + 256


def t5_bucket_np(n):
    n = np.asarray(n)
    max_exact = 16
    nf = np.maximum(n, 1).astype(np.float32)
    large = max_exact + (np.log(nf / max_exact) / math.log(128 / max_exact) * (32 - max_exact)).astype(np.int32)
    large = np.minimum(large, 31)
    return np.where(n < max_exact, n, large)


def host_consts(S, C):
    NT = S // 128
    c = {}
    c["ident"] = np.eye(128, dtype=np.float32)
    c["antiI"] = np.ascontiguousarray(np.eye(128, dtype=np.float32)[::-1])
    s = np.arange(128)[:, None]
    t = np.arange(128)[None, :]
    c["tri_incl"] = np.where(s <= t, -1.0 / 16, 0.0).astype(np.float32)
    c["tri_rev"] = np.where(s > t, -1.0 / 16, 0.0).astype(np.float32)
    c["maskT"] = (s <= t).astype(np.float32)
    c["lstrict"] = (s < t).astype(np.float32)
    c["ones"] = np.ones((128, 128), np.float32)
    r0 = np.zeros((128, 128), np.float32); r0[0, :] = 1.0
    c["row0"] = r0
    c["hmask3"] = (np.arange(128) >= 96).astype(np.float32).reshape(128, 1)
    c["hms4"] = ((np.arange(128)[:, None] // 32) == np.arange(4)[None, :]).astype(np.float32) * np.float32(32 ** -0.5)
    c["halfmask"] = ((np.arange(128)[:, None] // 64) == np.arange(2)[None, :]).astype(np.float32)
    log_g = np.log1p(-np.exp2(-5.0 - np.arange(4, dtype=np.float32))).astype(np.float32)
    rel = (t - s).astype(np.float32)
    DT = np.zeros((128, 4, 128), np.float32)
    for h in range(4):
        DT[:, h, :] = np.where(rel >= 0, np.exp(np.maximum(rel, 0) * log_g[h]), 0.0) * 0.125
    c["ret_DT"] = DT
    idx = np.arange(128, dtype=np.float32)
    c["ret_sd"] = (np.exp((127.0 - idx)[:, None] * log_g[None, :]) * 0.125).astype(np.float32)
    cd = np.zeros((128, 2, 128), np.float32)
    ch = np.zeros((128, 2), np.float32)
    for j in range(2):
        for half in range(2):
            h = 2 * j + half
            cd[64 * half:64 * half + 64, j, :] = np.exp((idx + 1.0) * log_g[h])[None, :]
            ch[64 * half:64 * half + 64, j] = np.exp(128.0 * log_g[h])
    c["ret_cdT"] = cd
    cdm = np.zeros((128, 2, 2, 128), np.float32)
    for half in range(2):
        cdm[64 * half:64 * half + 64, half, :, :] = cd[64 * half:64 * half + 64, :, :]
    c["ret_cdTm"] = cdm
    c["ret_chunk"] = ch
    pos = np.arange(S, dtype=np.float32)
    angle = (1.0 / (10000.0 ** np.linspace(0.0, 1.0, 32, dtype=np.float32))).astype(np.float32)
    angle = np.repeat(angle, 2)
    ang = (pos[:, None] * angle[None, :]).astype(np.float32)
    sin = np.sin(ang).astype(np.float32)
    cos = np.cos(ang).astype(np.float32)
    sgn = np.where(np.arange(64) % 2 == 0, -1.0, 1.0).astype(np.float32)
    cosT = np.concatenate([cos.T, cos.T], axis=0)
    sinsT = np.concatenate([(sin * sgn).T, (sin * sgn).T], axis=0)
    c["cosT"] = np.ascontiguousarray(cosT)
    c["sinsT"] = np.ascontiguousarray(sinsT)
    OH = np.zeros((33, 1152), np.float32)
    for i in range(1151):
        dist = i - 511
        if dist < 0:
            OH[32, i] = 1.0
        else:
            OH[int(t5_bucket_np(dist)), i] = 1.0
    c["t5oh"] = OH
    c["ecap"] = np.tile((np.arange(32, dtype=np.float32) * C + 1.0)[None, :], (128, 1))
    ts = np.zeros((128, NT, 4, 2), np.int32)
    tok = np.arange(128)[:, None] + 128 * np.arange(NT)[None, :]
    for j in range(4):
        ts[:, :, j, 0] = tok
        ts[:, :, j, 1] = 4 * tok + j
    c["tokslot"] = ts.reshape(128, NT * 8)
    li = np.zeros((32 * C, 2), np.int32)
    li[:, 0] = S
    li[:, 1] = 1 << 24
    c["list_init"] = li
    return c


CONST_SHAPES = None


def build(S, L, C, debug=(), lam_inits=None, stop=None, max_ops=None):
    NT = S // 128
    SC = min(512, S)
    NSC = S // SC
    TPS = SC // 128
    NR = C // 128
    nc = bass.Bass("TRN2", target_bir_lowering=False)
    P = Prog(nc)
    if lam_inits is None:
        lam_inits = [0.8 - 0.6 * math.exp(-0.3 * i) for i in range(L)]

    def din(name, shape, dt=F32):
        return nc.dram_tensor(name, list(shape), dt, kind="ExternalInput").ap()

    def dscr(name, shape, dt=F32):
        kind = "ExternalOutput" if name in debug else "Internal"
        return nc.dram_tensor(name, list(shape), dt, kind=kind).ap()

    x_in = din("x", [S, D])
    p_in = din("p", [L, S, PLE])
    w_in = din("w_in", [L, D, NCOL])
    wg_aug = din("wg_aug", [L, 17, 128])
    gng_in = din("gla_norm_g", [L, 64])
    dlam_in = din("diff_lambda", [L, 4, 64])
    dng_in = din("diff_norm_g", [L, 128])
    w_out = din("w_out", [L, D, D])
    rb_aug = din("rb_aug", [33, 4])
    ln1g = din("ln1_g", [L, D])
    ln1b = din("ln1_b", [L, D])
    w_router = din("w_router", [L, D, NE])
    b_router = din("b_router", [L, NE])
    w_gu = din("w_gu", [L, NE, D, 2 * D])
    b_gu = din("b_gu", [L, 128, NE, 16])
    w_down = din("w_down", [L, NE, D, D])
    b_down = din("b_down", [L, NE, D])
    w_pg = din("w_pg", [L, D, D])
    b_pg = din("b_pg", [L, D])
    w_pp = din("w_pp", [L, PLE, D])
    ln2g = din("ln2_g", [L, D])
    ln2b = din("ln2_b", [L, D])
    hc = host_consts(S, C)
    cin = {}
    for k, v in hc.items():
        cin[k] = din("c_" + k, v.shape, I32 if v.dtype == np.int32 else F32)
    out = nc.dram_tensor("out", [S, D], F32, kind="ExternalOutput").ap()

    xbuf = [dscr(f"xbuf{i}", [S, D]) for i in range(2)]
    zfm_f = dscr("zfm_f", [272, S])
    zfm_b = dscr("zfm_b", [1536, S], BF16)
    ztm_f = dscr("ztm_f", [S, 640])
    ztm_b = dscr("ztm_b", [S, 1024], BF16)
    mixed = dscr("mixed", [S, D], BF16)
    h_bf = dscr("h_bf", [S + 1, D], BF16)
    r2 = dscr("r2", [S, D])
    lst = dscr("lst", [NE * C, 2], I32)
    ybuf = dscr("ybuf", [4 * S, D])
    Fd = dscr("Fd", [4, 1152])

    SB_LIMIT = 229376 - 512
    sb_state = {"off": 16640, "n": 0, "base": 16640}

    def sb(name, shape, dt):
        esz = {F32: 4, BF16: 2, I32: 4}[dt]
        nbytes = int(np.prod(shape[1:])) * esz
        nbytes = (nbytes + 63) // 64 * 64
        off = sb_state["off"]
        assert off + nbytes <= SB_LIMIT, (name, off, nbytes)
        sb_state["off"] = off + nbytes
        sb_state["n"] += 1
        return nc.alloc_sbuf_tensor_at(f"{name}_{sb_state['n']}", list(shape), dt, offset=off)

    import os as _os
    def phase_begin():
        P.barrier()
        sb_state["pc"] = sb_state.get("pc", 0) + 1
        if not (_os.environ.get("NO_SB_RESET") and sb_state["pc"] == int(_os.environ.get("NO_SB_RESET"))):
            sb_state["off"] = sb_state["base"]

    ps = [nc.alloc_psum_tensor(f"ps{i}", [128, 512], F32) for i in range(8)]

    def PS(i):
        return f"ps{i}"

    ident_f = sb("ident_f", [128, 128], F32)
    ident_b = sb("ident_b", [128, 128], BF16)
    anti_f = sb("anti_f", [128, 128], F32)
    anti_b = sb("anti_b", [128, 128], BF16)
    ones_f = sb("ones_f", [128, 128], F32)
    ones_b = sb("ones_b", [128, 128], BF16)
    row0_f = sb("row0_f", [128, 128], F32)
    row0_b = sb("row0_b", [128, 128], BF16)
    tri_incl = sb("tri_incl", [128, 128], F32)
    tri_rev = sb("tri_rev", [128, 128], F32)
    maskT = sb("maskT", [128, 128], F32)
    lstrict = sb("lstrict", [128, 128], F32)
    ret_DT = sb("ret_DT", [128, 4, 128], F32)
    ret_sd = sb("ret_sd", [128, 4], F32)
    ret_cdT = sb("ret_cdT", [128, 2, 128], F32)
    ret_chunk = sb("ret_chunk", [128, 2], F32)
    ecap = sb("ecap", [128, 32], F32)
    hmask3 = sb("hmask3", [128, 1], F32)
    hms4 = sb("hms4", [128, 4], F32)
    halfmask = sb("halfmask", [128, 2], F32)
    ret_cdTm = sb("ret_cdTm", [128, 2, 2, 128], F32)
    tokslot = sb("tokslot", [128, NT * 8], I32)
    cfar = sb("cfar", [128, 4], F32)
    BTb = sb("BTb", [128, 4, 1024], BF16)
    g4all = sb("g4all", [128, NT, 4], F32)
    zero_b = sb("zero_b", [128, 1024], BF16)
    for nm, t_ in (("row0", row0_f), ("ident", ident_f), ("antiI", anti_f), ("ones", ones_f), ("tri_incl", tri_incl), ("tri_rev", tri_rev),
                   ("maskT", maskT), ("lstrict", lstrict), ("ret_DT", ret_DT), ("ret_sd", ret_sd),
                   ("ret_cdT", ret_cdT), ("ret_chunk", ret_chunk), ("ecap", ecap), ("hmask3", hmask3), ("hms4", hms4), ("halfmask", halfmask), ("ret_cdTm", ret_cdTm), ("tokslot", tokslot)):
        P.dma("sp", lambda e, nm=nm, t_=t_: e.dma_start(out=t_[:], in_=cin[nm]), writes=[nm])
    P.dve(lambda e: e.tensor_copy(out=ident_b[:], in_=ident_f[:]), reads=["ident"], writes=["ident_b"])
    P.dve(lambda e: e.tensor_copy(out=ones_b[:], in_=ones_f[:]), reads=["ones"], writes=["ones_b"])
    P.dve(lambda e: e.tensor_copy(out=row0_b[:], in_=row0_f[:]), reads=["row0"], writes=["row0_b"])
    P.dve(lambda e: e.tensor_copy(out=anti_b[:], in_=anti_f[:]), reads=["antiI"], writes=["anti_b"])
    P.dve(lambda e: e.memset(zero_b[:], 0.0), writes=["zero_b"])
    P.dma("sp", lambda e: e.dma_start(out=h_bf[S:S + 1, :], in_=zero_b[0:1, :]), reads=["zero_b"], writes=["h_bf_z"])
    sb_state["base"] = sb_state["off"]
    zero_f = sb("zero_f", [128, 2048], F32)
    BTf = sb("BTf", [128, 4, 1024], F32)
    P.dve(lambda e: e.memset(zero_f[:], 0.0), writes=["zero_f"])
    ybv = ybuf.rearrange("(a p r) d -> a p (r d)", p=128, r=2)
    for a in range(ybv.shape[0]):
        P.dma("sp" if a % 2 == 0 else "pool", lambda e, a=a: e.dma_start(out=ybv[a], in_=zero_f[:]),
              reads=["zero_f"], writes=[f"ybz{a}"])
    t5oh = sb("t5oh", [33, 1152], F32)
    rbs = sb("rbs", [33, 4], F32)
    Fs = sb("Fs", [4, 1152], F32)
    P.dma("sp", lambda e: e.dma_start(out=t5oh[:], in_=cin["t5oh"]), writes=["t5oh"])
    P.dma("sp", lambda e: e.dma_start(out=rbs[:], in_=rb_aug), writes=["rbs"])
    for i, (lo, n) in enumerate(((0, 512), (512, 512), (1024, 128))):
        P.pe(lambda e, i=i, lo=lo, n=n: e.matmul(ps[i][0:4, 0:n], lhsT=rbs[:, :], rhs=t5oh[:, lo:lo + n], start=True, stop=True),
             reads=["t5oh", "rbs"], writes=[PS(i)])
        P.dve(lambda e, i=i, lo=lo, n=n: e.tensor_copy(out=Fs[:, lo:lo + n], in_=ps[i][0:4, 0:n]), reads=[PS(i)], writes=["Fs"])
    P.dma("sp", lambda e: e.dma_start(out=Fd, in_=Fs[:]), reads=["Fs"], writes=["Fd"])
    for h in range(4):
        src = bass.AP(tensor=Fd.tensor, offset=h * 1152, ap=[[1, 128], [1, 1024]])
        P.dma("sp", lambda e, h=h, src=src: e.dma_start(out=BTf[:, h, :], in_=src), reads=["Fd"], writes=["BTf"])
    P.dve(lambda e: e.tensor_copy(out=BTb[:], in_=BTf[:]), reads=["BTf"], writes=["BTb"])
    P.dve(lambda e: e.tensor_copy(out=cfar[:], in_=BTf[:, :, 1023]), reads=["BTf"], writes=["cfar"])

    def rms_scale(o_ps_ap, nh, hd, sq, ss, rstd):
        pass

    def layer_norm(src, dst, gam, bet, tagp, st6, mv, lrs, xn):
        sname, dname = tagp
        for hb in range(2):
            P.dve(lambda e, hb=hb: e.bn_stats(out=st6[:, hb * 6:(hb + 1) * 6], in_=src[:, hb * 512:(hb + 1) * 512]),
                  reads=[sname], writes=["st6"])
        P.dve(lambda e: e.bn_aggr(out=mv[:], in_=st6[:]), reads=["st6"], writes=["mv"])
        P.dve(lambda e: e.tensor_scalar(out=lrs[:], in0=mv[:, 1:2], scalar1=LN_EPS, scalar2=None, op0=ALU.add),
              reads=["mv"], writes=["lrs"])
        P.act(lambda e: e.activation(out=lrs[:], in_=lrs[:], func=AF.Sqrt), reads=["lrs"], writes=["lrs"])
        P.dve(lambda e: e.reciprocal(out=lrs[:], in_=lrs[:]), reads=["lrs"], writes=["lrs"])
        P.dve(lambda e: e.tensor_scalar(out=xn[:], in0=src[:], scalar1=mv[:, 0:1], scalar2=lrs[:, 0:1], op0=ALU.subtract, op1=ALU.mult),
              reads=[sname, "mv", "lrs"], writes=["xn"])
        P.pool(lambda e: e.tensor_tensor(out=xn[:], in0=xn[:], in1=gam[:], op=ALU.mult), reads=["xn", "lng"], writes=["xn"])
        P.pool(lambda e: e.tensor_tensor(out=dst[:], in0=xn[:], in1=bet[:], op=ALU.add), reads=["xn", "lnb"], writes=[dname])

    def layer(l):
        X_in = x_in if l == 0 else xbuf[(l - 1) % 2]
        X_out = out if l == L - 1 else xbuf[l % 2]
        lam_init = lam_inits[l]
        if stop == ("pre", l):
            return True

        def _phase1():
            phase_begin()
            xT = sb("xT", [128, 8, S], BF16)
            win = sb("win", [128, 8, NCOL], BF16)
            xin = [sb(f"xin{i}", [128, D], F32) for i in range(2)]
            stf = [sb(f"stf{i}", [128, SC], F32) for i in range(4)]
            stb = [sb(f"stb{i}", [128, SC], BF16) for i in range(4)]
            cosb = [sb(f"cosb{i}", [128, SC], F32) for i in range(2)]
            sinb = [sb(f"sinb{i}", [128, SC], F32) for i in range(2)]
            rt1 = [sb(f"rt1{i}", [128, SC], F32) for i in range(2)]
            rt2 = [sb(f"rt2{i}", [128, SC], F32) for i in range(2)]
            tmf = [sb(f"tmf{i}", [128, 640], F32) for i in range(2)]
            tmb = [sb(f"tmb{i}", [128, 1024], BF16) for i in range(2)]
            for k in range(8):
                P.dma("pool", lambda e, k=k: e.dma_start(out=win[:, k, :], in_=w_in[l, k * 128:(k + 1) * 128, :]),
                      writes=[f"win{k}"])
            WIN = [f"win{k}" for k in range(8)]
            for t in range(NT):
                xb = xin[t % 2]
                P.dma("sp", lambda e, t=t, xb=xb: e.dma_start(out=xb[:], in_=X_in[t * 128:(t + 1) * 128, :]),
                      writes=[f"xin{t % 2}"])
                for hb in range(2):
                    bank = (t % 2) * 2 + hb
                    for kk in range(4):
                        k = hb * 4 + kk
                        P.pe(lambda e, bank=bank, kk=kk, k=k, xb=xb: e.matmul(
                            ps[bank][:, kk * 128:(kk + 1) * 128], lhsT=xb[:, k * 128:(k + 1) * 128], rhs=ident_f[:],
                            start=True, stop=True), reads=[f"xin{t % 2}", "ident"], writes=[PS(bank)])
                    eng = P.act if hb == 0 else P.dve
                    if hb == 0:
                        P.act(lambda e, bank=bank, t=t, hb=hb: e.activation(
                            out=xT[:, hb * 4:hb * 4 + 4, t * 128:(t + 1) * 128],
                            in_=ps[bank][:, :].rearrange("p (k n) -> p k n", k=4), func=AF.Copy),
                            reads=[PS(bank)], writes=[f"xT{t // TPS}"])
                    else:
                        P.dve(lambda e, bank=bank, t=t, hb=hb: e.tensor_copy(
                            out=xT[:, hb * 4:hb * 4 + 4, t * 128:(t + 1) * 128],
                            in_=ps[bank][:, :].rearrange("p (k n) -> p k n", k=4)),
                            reads=[PS(bank)], writes=[f"xT{t // TPS}"])
            bankc = [0]

            def nb():
                b = bankc[0] % 8
                bankc[0] += 1
                return b

            stc = [0]
            for sc in range(NSC):
                tok = slice(sc * SC, (sc + 1) * SC)
                XT = [f"xT{sc}"]

                def fm_mm(col0, ncols, bank):
                    for k in range(8):
                        P.pe(lambda e, k=k, col0=col0, ncols=ncols, bank=bank: e.matmul(
                            ps[bank][0:ncols, 0:SC], lhsT=win[:, k, col0:col0 + ncols], rhs=xT[:, k, tok],
                            start=(k == 0), stop=(k == 7)), reads=XT + [f"win{k}"], writes=[PS(bank)])

                for (col0, ncols, row0) in ((FM_GQ, 128, 0), (FM_GK, 128, 128), (FM_GLR, 16, 256)):
                    bank = nb()
                    fm_mm(col0, 128, bank)
                    si = stc[0] % 4
                    stc[0] += 1
                    P.act(lambda e, bank=bank, ncols=ncols, si=si: e.activation(out=stf[si][0:ncols, :], in_=ps[bank][0:ncols, 0:SC], func=AF.Copy),
                          reads=[PS(bank)], writes=[f"stf{si}"])
                    P.dma("sp", lambda e, si=si, ncols=ncols, row0=row0: e.dma_start(out=zfm_f[row0:row0 + ncols, tok], in_=stf[si][0:ncols, :]),
                          reads=[f"stf{si}"], writes=[f"zfm_f{sc}"])
                for i in range(8):
                    col0 = FM_DQ + i * 128
                    bank = nb()
                    fm_mm(col0, 128, bank)
                    si = stc[0] % 4
                    stc[0] += 1
                    scale = 0.125 if i < 4 else 1.0
                    if i % 2 == 0:
                        P.act(lambda e, bank=bank, si=si, scale=scale: e.activation(out=stb[si][:, :], in_=ps[bank][:, 0:SC], func=AF.Copy, scale=scale),
                              reads=[PS(bank)], writes=[f"stb{si}"])
                    else:
                        P.dve(lambda e, bank=bank, si=si, scale=scale: e.tensor_scalar(out=stb[si][:, :], in0=ps[bank][:, 0:SC], scalar1=scale, scalar2=None, op0=ALU.mult),
                              reads=[PS(bank)], writes=[f"stb{si}"])
                    P.dma("sp", lambda e, si=si, i=i: e.dma_start(out=zfm_b[i * 128:(i + 1) * 128, tok], in_=stb[si][:, :]),
                          reads=[f"stb{si}"], writes=[f"zfm_b{sc}"])
                cb = sc % 2
                P.dma("sp", lambda e, cb=cb: e.dma_start(out=cosb[cb][:], in_=cin["cosT"][:, tok]), writes=[f"cosb{cb}"])
                P.dma("sp", lambda e, cb=cb: e.dma_start(out=sinb[cb][:], in_=cin["sinsT"][:, tok]), writes=[f"sinb{cb}"])
                ri = 0
                for (c_a, c_s, row0) in ((FM_RQ, FM_RQS, 1024), (FM_RK, FM_RKS, 1280)):
                    for j in range(2):
                        b1 = nb()
                        fm_mm(c_a + j * 128, 128, b1)
                        b2 = nb()
                        fm_mm(c_s + j * 128, 128, b2)
                        r_ = ri % 2
                        ri += 1
                        si = stc[0] % 4
                        stc[0] += 1
                        P.dve(lambda e, b1=b1, r_=r_, cb=cb: e.tensor_tensor(out=rt1[r_][:], in0=ps[b1][:, 0:SC], in1=cosb[cb][:], op=ALU.mult),
                              reads=[PS(b1), f"cosb{cb}"], writes=[f"rt1{r_}"])
                        P.dve(lambda e, b2=b2, r_=r_, cb=cb: e.tensor_tensor(out=rt2[r_][:], in0=ps[b2][:, 0:SC], in1=sinb[cb][:], op=ALU.mult),
                              reads=[PS(b2), f"sinb{cb}"], writes=[f"rt2{r_}"])
                        P.pool(lambda e, r_=r_, si=si: e.tensor_tensor(out=stb[si][:], in0=rt1[r_][:], in1=rt2[r_][:], op=ALU.add),
                               reads=[f"rt1{r_}", f"rt2{r_}"], writes=[f"stb{si}"])
                        P.dma("sp", lambda e, si=si, row0=row0, j=j: e.dma_start(out=zfm_b[row0 + j * 128:row0 + (j + 1) * 128, tok], in_=stb[si][:, :]),
                              reads=[f"stb{si}"], writes=[f"zfm_b{sc}"])
                for tt in range(TPS):
                    t = sc * TPS + tt
                    tsl = slice(t * 128, (t + 1) * 128)
                    ti = t % 2
                    banks = []
                    for (col0, ncols) in ((TM0, 384), (TM1, 512), (TM2, 512), (TM3, 256)):
                        bank = nb()
                        banks.append(bank)
                        for k in range(8):
                            P.pe(lambda e, k=k, col0=col0, ncols=ncols, bank=bank, tsl=tsl: e.matmul(
                                ps[bank][:, 0:ncols], lhsT=xT[:, k, tsl], rhs=win[:, k, col0:col0 + ncols],
                                start=(k == 0), stop=(k == 7)), reads=XT + [f"win{k}"], writes=[PS(bank)])
                    b0, b1, b2, b3 = banks
                    P.act(lambda e, b0=b0, ti=ti: e.activation(out=tmf[ti][:, 0:128], in_=ps[b0][:, 0:128], func=AF.Copy),
                          reads=[PS(b0)], writes=[f"tmf{ti}"])
                    P.dve(lambda e, b0=b0, ti=ti: e.tensor_copy(out=tmb[ti][:, 0:256], in_=ps[b0][:, 128:384]),
                          reads=[PS(b0)], writes=[f"tmb{ti}"])
                    P.act(lambda e, b1=b1, ti=ti: e.activation(out=tmf[ti][:, 128:640], in_=ps[b1][:, 0:512], func=AF.Sigmoid),
                          reads=[PS(b1)], writes=[f"tmf{ti}"])
                    P.dve(lambda e, b1=b1, ti=ti: e.tensor_tensor(out=tmf[ti][:, 128:640], in0=tmf[ti][:, 128:640], in1=ps[b1][:, 0:512], op=ALU.mult),
                          reads=[PS(b1), f"tmf{ti}"], writes=[f"tmf{ti}"])
                    P.dve(lambda e, b2=b2, ti=ti: e.tensor_copy(out=tmb[ti][:, 256:768], in_=ps[b2][:, 0:512]),
                          reads=[PS(b2)], writes=[f"tmb{ti}"])
                    P.act(lambda e, b3=b3, ti=ti: e.activation(out=tmb[ti][:, 768:1024], in_=ps[b3][:, 0:256], func=AF.Copy),
                          reads=[PS(b3)], writes=[f"tmb{ti}"])
                    P.dma("sp", lambda e, ti=ti, tsl=tsl: e.dma_start(out=ztm_f[tsl, :], in_=tmf[ti][:]), reads=[f"tmf{ti}"], writes=[f"ztm_f{t}"])
                    P.dma("sp", lambda e, ti=ti, tsl=tsl: e.dma_start(out=ztm_b[tsl, :], in_=tmb[ti][:]), reads=[f"tmb{ti}"], writes=[f"ztm_b{t}"])
        _phase1()
        if stop == ("inproj", l):
            return True

        def _phase2():
            phase_begin()
            gqT = sb("gqT", [128, S], F32)
            gkT = sb("gkT", [128, S], F32)
            glra = sb("glra", [128, S], F32)
            gk_tm = sb("gk_tm", [128, NT, 128], F32)
            gv = sb("gv", [128, NT, 256], BF16)
            gsil = sb("gsil", [128, NT, 256], F32)
            mixg = sb("mixg", [128, NT, 256], BF16)
            wga = sb("wga", [128, 128], F32)
            gng = sb("gng", [128, 64], F32)
            Sf = sb("Sf", [128, 256], F32)
            Sb = sb("Sb", [128, 256], BF16)
            e1 = sb("e1", [128, 128], F32)
            spt = sb("spt", [128, 128], F32)
            eq = sb("eq", [128, 128], F32)
            ek = sb("ek", [128, 128], F32)
            er = sb("er", [128, 128], F32)
            qp = sb("qp", [128, 128], BF16)
            kp = sb("kp", [128, 128], BF16)
            kpp = sb("kpp", [128, 128], BF16)
            qpm = sb("qpm", [128, 4, 128], BF16)
            AT = sb("AT", [128, 4, 128], BF16)
            sq = sb("sq", [128, 256], F32)
            ss = sb("ss", [128, 4], F32)
            rstd = sb("rstd", [128, 4], F32)
            on = sb("on", [128, 4, 64], F32)
            on2 = sb("on2", [128, 4, 64], F32)
            P.dve(lambda e: e.memset(glra[:, :], 0.0), writes=["glra"])
            P.dve(lambda e: e.memset(glra[0:32, :], 1.0), writes=["glra"])
            P.dve(lambda e: e.memset(wga[:, :], 0.0), writes=["wga"])
            P.dma("sp", lambda e: e.dma_start(out=gqT[:], in_=zfm_f[0:128, :]), writes=["gqT"])
            P.dma("sp", lambda e: e.dma_start(out=gkT[:], in_=zfm_f[128:256, :]), writes=["gkT"])
            P.dma("sp", lambda e: e.dma_start(out=glra[0:16, :], in_=zfm_f[256:272, :]), writes=["glra"])
            P.dma("sp", lambda e: e.dma_start(out=gk_tm[:], in_=ztm_f[:, 0:128].rearrange("(t p) c -> p t c", p=128)), writes=["gk_tm"])
            P.dma("sp", lambda e: e.dma_start(out=gsil[:], in_=ztm_f[:, 128:384].rearrange("(t p) c -> p t c", p=128)), writes=["gsil"])
            P.dma("sp", lambda e: e.dma_start(out=gv[:], in_=ztm_b[:, 0:256].rearrange("(t p) c -> p t c", p=128)), writes=["gv"])
            P.dma("sp", lambda e: e.dma_start(out=wga[0:17, :], in_=wg_aug[l]), writes=["wga"])
            P.dma("sp", lambda e: e.dma_start(out=gng[:], in_=gng_in[l:l + 1, :].partition_broadcast(128)), writes=["gng"])
            P.dve(lambda e: e.memset(Sf[:], 0.0), writes=["Sf"])
            P.dve(lambda e: e.memset(Sb[:], 0.0), writes=["Sb"])
            for c in range(NT):
                ck = slice(c * 128, (c + 1) * 128)
                P.pe(lambda e, ck=ck: e.matmul(ps[0][:, 0:128], lhsT=glra[:, ck], rhs=wga[:, :], start=True, stop=True),
                     reads=["glra", "wga"], writes=[PS(0)])
                P.act(lambda e: e.activation(out=e1[:], in_=ps[0][:, 0:128], func=AF.Exp, scale=-1.0), reads=[PS(0)], writes=["e1"])
                P.act(lambda e: e.activation(out=spt[:], in_=e1[:], func=AF.Ln, bias=1.0), reads=["e1"], writes=["spt"])
                P.pe(lambda e: e.matmul(ps[1][:, 0:128], lhsT=spt[:], rhs=tri_incl[:], start=True, stop=True),
                     reads=["spt", "tri_incl"], writes=[PS(1)])
                P.pe(lambda e: e.matmul(ps[1][:, 128:256], lhsT=tri_rev[:], rhs=spt[:], start=True, stop=True),
                     reads=["spt", "tri_rev"], writes=[PS(1)])
                P.act(lambda e: e.activation(out=eq[:], in_=ps[1][:, 0:128], func=AF.Exp), reads=[PS(1)], writes=["eq"])
                P.act(lambda e: e.activation(out=ek[:], in_=ps[1][:, 0:128], func=AF.Exp, scale=-1.0), reads=[PS(1)], writes=["ek"])
                P.act(lambda e: e.activation(out=er[:], in_=ps[1][:, 128:256], func=AF.Exp), reads=[PS(1)], writes=["er"])
                for h in range(4):
                    P.dve(lambda e, ck=ck, h=h: e.scalar_tensor_tensor(out=qpm[:, h, :], in0=gqT[:, ck], scalar=hms4[:, h:h + 1], in1=eq[:],
                                                                     op0=ALU.mult, op1=ALU.mult), reads=["gqT", "eq", "hms4"], writes=["qpm"])
                P.dve(lambda e, ck=ck: e.tensor_tensor(out=kp[:], in0=gkT[:, ck], in1=ek[:], op=ALU.mult), reads=["gkT", "ek"], writes=["kp"])
                P.dve(lambda e, c=c: e.tensor_tensor(out=kpp[:], in0=gk_tm[:, c, :], in1=er[:], op=ALU.mult), reads=["gk_tm", "er"], writes=["kpp"])
                for h in range(4):
                    P.pe(lambda e, h=h: e.matmul(ps[2][:, h * 128:(h + 1) * 128], lhsT=kp[:, :], rhs=qpm[:, h, :],
                                                start=True, stop=True), reads=["kp", "qpm"], writes=[PS(2)])
                P.dve(lambda e: e.tensor_tensor(out=AT[:], in0=ps[2][:, :].rearrange("p (h t) -> p h t", h=4),
                                                in1=maskT[:, :].unsqueeze(1).to_broadcast([128, 4, 128]), op=ALU.mult),
                      reads=[PS(2), "maskT"], writes=["AT"])
                P.pe(lambda e, c=c: e.matmul(ps[3][:, 0:256], lhsT=kpp[:], rhs=gv[:, c, :], start=True, stop=True),
                     reads=["kpp", "gv"], writes=[PS(3)])
                for h in range(4):
                    P.pe(lambda e, h=h, c=c: e.matmul(ps[4][:, h * 64:(h + 1) * 64], lhsT=AT[:, h, :], rhs=gv[:, c, h * 64:(h + 1) * 64],
                                                      start=True, stop=False), reads=["AT", "gv"], writes=[PS(4)])
                    P.pe(lambda e, h=h: e.matmul(ps[4][:, h * 64:(h + 1) * 64], lhsT=qpm[:, h, :],
                                                rhs=Sb[:, 64 * h:64 * h + 64], start=False, stop=True),
                         reads=["qpm", "Sb"], writes=[PS(4)])
                P.dve(lambda e: e.scalar_tensor_tensor(out=Sf[:], in0=Sf[:], scalar=eq[:, 127:128], in1=ps[3][:, 0:256], op0=ALU.mult, op1=ALU.add),
                      reads=["Sf", "eq", PS(3)], writes=["Sf"])
                P.act(lambda e: e.activation(out=Sb[:], in_=Sf[:], func=AF.Copy), reads=["Sf"], writes=["Sb"])
                P.act(lambda e: e.activation(out=sq[:], in_=ps[4][:, 0:256], func=AF.Square), reads=[PS(4)], writes=["sq"])
                P.dve(lambda e: e.tensor_reduce(out=ss[:], in_=sq[:, :].rearrange("p (h v) -> p h v", h=4), axis=AX.X, op=ALU.add),
                      reads=["sq"], writes=["ss"])
                P.dve(lambda e: e.tensor_scalar(out=rstd[:], in0=ss[:], scalar1=1.0 / 64, scalar2=HN_EPS, op0=ALU.mult, op1=ALU.add),
                      reads=["ss"], writes=["rstd"])
                P.act(lambda e: e.activation(out=rstd[:], in_=rstd[:], func=AF.Sqrt), reads=["rstd"], writes=["rstd"])
                P.dve(lambda e: e.reciprocal(out=rstd[:], in_=rstd[:]), reads=["rstd"], writes=["rstd"])
                P.dve(lambda e: e.tensor_tensor(out=on[:], in0=ps[4][:, 0:256].rearrange("p (h v) -> p h v", h=4),
                                                in1=rstd[:, :].unsqueeze(2).to_broadcast([128, 4, 64]), op=ALU.mult),
                      reads=[PS(4), "rstd"], writes=["on"])
                P.pool(lambda e: e.tensor_tensor(out=on2[:], in0=on[:], in1=gng[:, :].unsqueeze(1).to_broadcast([128, 4, 64]), op=ALU.mult),
                       reads=["on", "gng"], writes=["on2"])
                P.pool(lambda e, c=c: e.tensor_tensor(out=mixg[:, c, :], in0=on2[:, :, :].rearrange("p h v -> p (h v)"), in1=gsil[:, c, :], op=ALU.mult),
                       reads=["on2", "gsil"], writes=["mixg"])
            P.dma("sp", lambda e: e.dma_start(out=mixed[:, 0:256].rearrange("(t p) c -> p t c", p=128), in_=mixg[:]),
                  reads=["mixg"], writes=["mixed_g"])
        _phase2()
        if stop == ("gla", l):
            return True

        def _phase3():
            phase_begin()
            rqT = sb("rqT", [128, 2, S], BF16)
            rkT = sb("rkT", [128, 2, S], BF16)
            rv = sb("rv", [128, NT, 256], BF16)
            rsil = sb("rsil", [128, NT, 256], F32)
            mixr = sb("mixr", [128, NT, 256], BF16)
            RSf = sb("RSf", [128, 2, 128], F32)
            RSb = sb("RSb", [128, 2, 128], BF16)
            RAT = sb("RAT", [128, 4, 128], BF16)
            kd = sb("kd", [128, 4, 64], BF16)
            rqm = sb("rqm", [128, 2, 2, 128], BF16)
            rqcm = sb("rqcm", [128, 2, 2, 128], BF16)
            sq = sb("rsq", [128, 256], F32)
            ss = sb("rss", [128, 4], F32)
            rstd = sb("rrstd", [128, 4], F32)
            on = sb("ron", [128, 4, 64], F32)
            for j in range(2):
                P.dma("sp", lambda e, j=j: e.dma_start(out=rqT[:, j, :], in_=zfm_b[1024 + j * 128:1024 + (j + 1) * 128, :]), writes=["rqT"])
                P.dma("sp", lambda e, j=j: e.dma_start(out=rkT[:, j, :], in_=zfm_b[1280 + j * 128:1280 + (j + 1) * 128, :]), writes=["rkT"])
            P.dma("sp", lambda e: e.dma_start(out=rv[:], in_=ztm_b[:, 768:1024].rearrange("(t p) c -> p t c", p=128)), writes=["rv"])
            P.dma("sp", lambda e: e.dma_start(out=rsil[:], in_=ztm_f[:, 384:640].rearrange("(t p) c -> p t c", p=128)), writes=["rsil"])
            P.dve(lambda e: e.memset(RSf[:], 0.0), writes=["RSf"])
            P.dve(lambda e: e.memset(RSb[:], 0.0), writes=["RSb"])
            for c in range(NT):
                ck = slice(c * 128, (c + 1) * 128)
                for half in range(2):
                    P.dve(lambda e, half=half, ck=ck: e.tensor_scalar(out=rqm[:, half, :, :], in0=rqT[:, :, ck], scalar1=halfmask[:, half:half + 1],
                                                                     scalar2=None, op0=ALU.mult), reads=["rqT", "halfmask"], writes=["rqm"])
                    P.dve(lambda e, half=half, ck=ck: e.tensor_tensor(out=rqcm[:, half, :, :], in0=rqT[:, :, ck], in1=ret_cdTm[:, half, :, :], op=ALU.mult),
                          reads=["rqT", "ret_cdTm"], writes=["rqcm"])
                for h in range(4):
                    j, half = h // 2, h % 2
                    pr = slice(64 * half, 64 * half + 64)
                    P.pe(lambda e, h=h, j=j, half=half, ck=ck: e.matmul(ps[0][:, h * 128:(h + 1) * 128], lhsT=rkT[:, j, ck], rhs=rqm[:, half, j, :],
                                                                     start=True, stop=True), reads=["rkT", "rqm"], writes=[PS(0)])
                P.dve(lambda e: e.tensor_tensor(out=RAT[:], in0=ps[0][:, :].rearrange("p (h t) -> p h t", h=4), in1=ret_DT[:], op=ALU.mult),
                      reads=[PS(0), "ret_DT"], writes=["RAT"])
                for j in range(2):
                    P.pe(lambda e, j=j, ck=ck: e.matmul(ps[1][:, j * 128:(j + 1) * 128], lhsT=rkT[:, j, ck], rhs=ident_b[:], start=True, stop=True),
                         reads=["rkT", "ident_b"], writes=[PS(1)])
                P.dve(lambda e: e.tensor_tensor(out=kd[:], in0=ps[1][:, 0:256].rearrange("p (h d) -> p h d", h=4),
                                                in1=ret_sd[:, :].unsqueeze(2).to_broadcast([128, 4, 64]), op=ALU.mult),
                      reads=[PS(1), "ret_sd"], writes=["kd"])
                for j in range(2):
                    P.pe(lambda e, j=j, c=c: e.matmul(ps[2][:, j * 128:(j + 1) * 128], lhsT=kd[:, 2 * j:2 * j + 2, :].rearrange("p h d -> p (h d)"),
                                                      rhs=rv[:, c, j * 128:(j + 1) * 128], start=True, stop=True),
                         reads=["kd", "rv"], writes=[PS(2)])
                for h in range(4):
                    j, half = h // 2, h % 2
                    pr = slice(64 * half, 64 * half + 64)
                    P.pe(lambda e, h=h, c=c: e.matmul(ps[3][:, h * 64:(h + 1) * 64], lhsT=RAT[:, h, :], rhs=rv[:, c, h * 64:(h + 1) * 64],
                                                      start=True, stop=False), reads=["RAT", "rv"], writes=[PS(3)])
                    P.pe(lambda e, h=h, j=j, half=half: e.matmul(ps[3][:, h * 64:(h + 1) * 64], lhsT=rqcm[:, half, j, :],
                                                              rhs=RSb[:, j, 64 * half:64 * half + 64], start=False, stop=True),
                         reads=["rqcm", "RSb"], writes=[PS(3)])
                for j in range(2):
                    P.dve(lambda e, j=j: e.scalar_tensor_tensor(out=RSf[:, j, :], in0=RSf[:, j, :], scalar=ret_chunk[:, j:j + 1],
                                                                in1=ps[2][:, j * 128:(j + 1) * 128], op0=ALU.mult, op1=ALU.add),
                          reads=["RSf", "ret_chunk", PS(2)], writes=["RSf"])
                P.act(lambda e: e.activation(out=RSb[:], in_=RSf[:], func=AF.Copy), reads=["RSf"], writes=["RSb"])
                P.act(lambda e: e.activation(out=sq[:], in_=ps[3][:, 0:256], func=AF.Square), reads=[PS(3)], writes=["rsq"])
                P.dve(lambda e: e.tensor_reduce(out=ss[:], in_=sq[:, :].rearrange("p (h v) -> p h v", h=4), axis=AX.X, op=ALU.add),
                      reads=["rsq"], writes=["rss"])
                P.dve(lambda e: e.tensor_scalar(out=rstd[:], in0=ss[:], scalar1=1.0 / 64, scalar2=HN_EPS, op0=ALU.mult, op1=ALU.add),
                      reads=["rss"], writes=["rrstd"])
                P.act(lambda e: e.activation(out=rstd[:], in_=rstd[:], func=AF.Sqrt), reads=["rrstd"], writes=["rrstd"])
                P.dve(lambda e: e.reciprocal(out=rstd[:], in_=rstd[:]), reads=["rrstd"], writes=["rrstd"])
                P.dve(lambda e: e.tensor_tensor(out=on[:], in0=ps[3][:, 0:256].rearrange("p (h v) -> p h v", h=4),
                                                in1=rstd[:, :].unsqueeze(2).to_broadcast([128, 4, 64]), op=ALU.mult),
                      reads=[PS(3), "rrstd"], writes=["ron"])
                P.pool(lambda e, c=c: e.tensor_tensor(out=mixr[:, c, :], in0=on[:, :, :].rearrange("p h v -> p (h v)"), in1=rsil[:, c, :], op=ALU.mult),
                       reads=["ron", "rsil"], writes=["mixr"])
            P.dma("sp", lambda e: e.dma_start(out=mixed[:, 768:1024].rearrange("(t p) c -> p t c", p=128), in_=mixr[:]),
                  reads=["mixr"], writes=["mixed_r"])
        _phase3()
        if stop == ("ret", l):
            return True

        def _phase4():
            phase_begin()
            dqh = [sb(f"dqh{i}", [128, S], BF16) for i in range(2)]
            dqm = [sb(f"dqm{i}", [128, 2, S], BF16) for i in range(2)]
            dkh = [sb(f"dkh{i}", [128, S], BF16) for i in range(2)]
            V1 = [sb(f"V1{i}", [128, NT, 129], BF16) for i in range(2)]
            mixd = sb("mixd", [128, NT, 512], BF16)
            pT = [[sb(f"pT{a}{m}", [128, SC], BF16) for m in range(2)] for a in range(2)]
            dl = sb("dl", [1, 4, 64], F32)
            prod = sb("prod", [1, 2, 64], F32)
            s2 = sb("s2", [1, 2], F32)
            e2 = sb("e2", [1, 2], F32)
            nlam1 = sb("nlam1", [128, 1], F32)
            nlam = sb("nlam", [128, 1], F32)
            dngs = sb("dngs", [128, 128], F32)
            rc = sb("rc", [128, 8], F32)
            a1 = sb("a1", [128, 128], F32)
            dh = sb("dh", [128, 4, 128], F32)
            dsq = sb("dsq", [128, 4, 128], F32)
            dss = sb("dss", [128, 4], F32)
            drs = sb("drs", [128, 4], F32)
            dn = sb("dn", [128, 4, 128], F32)
            for i in range(2):
                P.dve(lambda e, i=i: e.memset(V1[i][:], 1.0), writes=[f"V1{i}"])
            P.dma("sp", lambda e: e.dma_start(out=dl[:], in_=dlam_in[l:l + 1, :, :]), writes=["dl"])
            P.dma("sp", lambda e: e.dma_start(out=dngs[:], in_=dng_in[l:l + 1, :].partition_broadcast(128)), writes=["dngs"])
            P.dve(lambda e: e.tensor_scalar(out=dngs[:], in0=dngs[:], scalar1=1.0 - lam_init, scalar2=None, op0=ALU.mult), reads=["dngs"], writes=["dngs"])
            P.dve(lambda e: e.tensor_tensor(out=prod[:], in0=dl[:, 0:4:2, :], in1=dl[:, 1:4:2, :], op=ALU.mult), reads=["dl"], writes=["prod"])
            P.dve(lambda e: e.tensor_reduce(out=s2[:], in_=prod[:], axis=AX.X, op=ALU.add), reads=["prod"], writes=["s2"])
            P.act(lambda e: e.activation(out=e2[:], in_=s2[:], func=AF.Exp), reads=["s2"], writes=["e2"])
            P.dve(lambda e: e.memset(nlam1[:], 0.0), writes=["nlam1"])
            P.dve(lambda e: e.tensor_tensor(out=nlam1[0:1, :], in0=e2[:, 1:2], in1=e2[:, 0:1], op=ALU.subtract), reads=["e2"], writes=["nlam1"])
            P.dve(lambda e: e.tensor_scalar(out=nlam1[0:1, :], in0=nlam1[0:1, :], scalar1=-lam_init, scalar2=None, op0=ALU.add), reads=["nlam1"], writes=["nlam1"])
            P.pe(lambda e: e.matmul(ps[7][:, 0:1], lhsT=ones_f[:, :], rhs=nlam1[:, 0:1], start=True, stop=True), reads=["nlam1", "ones"], writes=[PS(7)])
            P.dve(lambda e: e.tensor_copy(out=nlam[:], in_=ps[7][:, 0:1]), reads=[PS(7)], writes=["nlam"])

            def acc_ap(m, sub):
                idx = m * 4 + sub
                return ps[4 + idx // 3], 4 + idx // 3, (idx % 3) * 129

            cnt = 0
            QT = S // SC
            SUBS = SC // 128
            for h in range(4):
                hi = h % 2
                P.dma("sp", lambda e, h=h, hi=hi: e.dma_start(out=dqh[hi][:], in_=zfm_b[h * 128:(h + 1) * 128, :]), writes=[f"dqh{hi}"])
                P.dma("sp", lambda e, h=h, hi=hi: e.dma_start(out=dkh[hi][:], in_=zfm_b[512 + h * 128:512 + (h + 1) * 128, :]), writes=[f"dk{hi}"])
                P.dma("sp", lambda e, h=h, hi=hi: e.dma_start(out=V1[hi][:, :, 0:128],
                                                              in_=ztm_b[:, 256 + h * 128:256 + (h + 1) * 128].rearrange("(t p) c -> p t c", p=128)),
                      writes=[f"V1{hi}"])
                for m in range(2):
                    P.dve(lambda e, hi=hi, m=m: e.tensor_scalar(out=dqm[hi][:, m, :], in0=dqh[hi][:], scalar1=halfmask[:, m:m + 1], scalar2=None, op0=ALU.mult),
                          reads=[f"dqh{hi}", "halfmask"], writes=[f"dq{hi}"])
                for jq in range(QT):
                    for b in (4, 5, 6):
                        P.dve(lambda e, b=b: e.memset(ps[b][:, :], 0.0), writes=[PS(b)])
                    nkb = SUBS * jq + SUBS
                    for kb in range(nkb):
                        i = kb - SUBS * jq
                        qlo = max(0, 128 * i)
                        n = SC - qlo
                        a = cnt % 2
                        cnt += 1
                        ksl = slice(kb * 128, (kb + 1) * 128)
                        for m in range(2):
                            bank = 2 * a + m
                            pr = slice(64 * m, 64 * m + 64)
                            near = i >= -1
                            P.pe(lambda e, bank=bank, m=m, hi=hi, ksl=ksl, qlo=qlo, jq=jq, near=near: e.matmul(
                                ps[bank][:, qlo:SC], lhsT=dkh[hi][:, ksl], rhs=dqm[hi][:, m, jq * SC + qlo:(jq + 1) * SC],
                                start=True, stop=not near), reads=[f"dq{hi}", f"dk{hi}"], writes=[PS(bank)])
                            if near:
                                c0 = 384 - 128 * i + qlo
                                P.pe(lambda e, bank=bank, h=h, qlo=qlo, c0=c0, n=n: e.matmul(
                                    ps[bank][:, qlo:SC], lhsT=anti_b[:], rhs=BTb[:, h, c0:c0 + n], start=False, stop=True),
                                    reads=["anti_b", "BTb"], writes=[PS(bank)])
                                P.act(lambda e, a=a, m=m, bank=bank, qlo=qlo: e.activation(out=pT[a][m][:, qlo:SC], in_=ps[bank][:, qlo:SC], func=AF.Exp),
                                      reads=[PS(bank)], writes=[f"pT{a}{m}"])
                            else:
                                P.act(lambda e, a=a, m=m, bank=bank, h=h: e.activation(out=pT[a][m][:, :], in_=ps[bank][:, 0:SC], func=AF.Exp,
                                                                                     bias=cfar[:, h:h + 1]),
                                      reads=[PS(bank), "cfar"], writes=[f"pT{a}{m}"])
                            for sub in range(qlo // 128, SUBS):
                                pst, bi, off = acc_ap(m, sub)
                                P.pe(lambda e, pst=pst, off=off, a=a, m=m, sub=sub, kb=kb, hi=hi: e.matmul(
                                    pst[:, off:off + 129], lhsT=pT[a][m][:, sub * 128:(sub + 1) * 128], rhs=V1[hi][:, kb, :],
                                    start=False, stop=False, skip_group_check=True),
                                    reads=[f"pT{a}{m}", f"V1{hi}"], writes=[PS(bi)])
                    for m in range(2):
                        for sub in range(SUBS):
                            pst, bi, off = acc_ap(m, sub)
                            P.dve(lambda e, pst=pst, off=off, m=m, sub=sub: e.reciprocal(out=rc[:, m * 4 + sub:m * 4 + sub + 1], in_=pst[:, off + 128:off + 129]),
                                  reads=[PS(bi)], writes=["rc"])
                    P.dve(lambda e: e.tensor_scalar(out=rc[:, 4:8], in0=rc[:, 4:8], scalar1=nlam[:, 0:1], scalar2=None, op0=ALU.mult),
                          reads=["rc", "nlam"], writes=["rc"])
                    for sub in range(SUBS):
                        p1, b1, o1 = acc_ap(0, sub)
                        p2, b2, o2 = acc_ap(1, sub)
                        P.act(lambda e, p1=p1, o1=o1, sub=sub: e.activation(out=a1[:], in_=p1[:, o1:o1 + 128], func=AF.Copy, scale=rc[:, sub:sub + 1]),
                              reads=[PS(b1), "rc"], writes=["a1"])
                        P.dve(lambda e, p2=p2, o2=o2, sub=sub: e.scalar_tensor_tensor(out=dh[:, sub, :], in0=p2[:, o2:o2 + 128], scalar=rc[:, 4 + sub:5 + sub],
                                                                                   in1=a1[:], op0=ALU.mult, op1=ALU.add),
                              reads=[PS(b2), "rc", "a1"], writes=["dh"])
                    P.act(lambda e: e.activation(out=dsq[:, 0:SUBS, :], in_=dh[:, 0:SUBS, :], func=AF.Square), reads=["dh"], writes=["dsq"])
                    P.dve(lambda e: e.tensor_reduce(out=dss[:, 0:SUBS], in_=dsq[:, 0:SUBS, :], axis=AX.X, op=ALU.add), reads=["dsq"], writes=["dss"])
                    P.dve(lambda e: e.tensor_scalar(out=drs[:, 0:SUBS], in0=dss[:, 0:SUBS], scalar1=1.0 / 128, scalar2=HN_EPS, op0=ALU.mult, op1=ALU.add),
                          reads=["dss"], writes=["drs"])
                    P.act(lambda e: e.activation(out=drs[:, 0:SUBS], in_=drs[:, 0:SUBS], func=AF.Sqrt), reads=["drs"], writes=["drs"])
                    P.dve(lambda e: e.reciprocal(out=drs[:, 0:SUBS], in_=drs[:, 0:SUBS]), reads=["drs"], writes=["drs"])
                    P.dve(lambda e: e.tensor_tensor(out=dn[:, 0:SUBS, :], in0=dh[:, 0:SUBS, :],
                                                    in1=drs[:, 0:SUBS].unsqueeze(2).to_broadcast([128, SUBS, 128]), op=ALU.mult),
                          reads=["dh", "drs"], writes=["dn"])
                    P.pool(lambda e, jq=jq, h=h: e.tensor_tensor(out=mixd[:, jq * SUBS:(jq + 1) * SUBS, h * 128:(h + 1) * 128], in0=dn[:, 0:SUBS, :],
                                                                 in1=dngs[:, :].unsqueeze(1).to_broadcast([128, SUBS, 128]), op=ALU.mult),
                           reads=["dn", "dngs"], writes=["mixd"])
            P.dma("sp", lambda e: e.dma_start(out=mixed[:, 256:768].rearrange("(t p) c -> p t c", p=128), in_=mixd[:]),
                  reads=["mixd"], writes=["mixed_d"])
        _phase4()
        if stop == ("diff", l):
            return True

        def _phase5():
            phase_begin()
            wout = sb("wout", [128, 8, D], BF16)
            wpg = sb("wpg", [128, 8, D], BF16)
            wpp = sb("wpp", [128, 2, D], BF16)
            wr = sb("wr", [128, 8, NE], F32)
            l1g = sb("l1g", [128, D], F32)
            l1b = sb("l1b", [128, D], F32)
            bpg = sb("bpg", [128, D], BF16)
            br = sb("br", [128, NE], F32)
            Macc = sb("Macc", [128, NE], F32)
            mt = [sb(f"mt{i}", [128, D], BF16) for i in range(2)]
            xi = [sb(f"xi{i}", [128, D], F32) for i in range(2)]
            pi = [sb(f"pi{i}", [128, PLE], F32) for i in range(2)]
            mT = sb("mT", [128, 8, 128], BF16)
            y = sb("y", [128, D], F32)
            st6 = sb("st6", [128, 12], F32)
            mv = sb("mv", [128, 2], F32)
            lrs = sb("lrs", [128, 1], F32)
            xn = sb("xn", [128, D], F32)
            x1 = [sb(f"x1{i}", [128, D], F32) for i in range(2)]
            x1b = [sb(f"x1b{i}", [128, D], BF16) for i in range(2)]
            x1Tf = sb("x1Tf", [128, 8, 128], F32)
            x1Tb = sb("x1Tb", [128, 8, 128], BF16)
            pTt = sb("pTt", [128, 2, 128], BF16)
            lg = sb("lg", [128, NE], F32)
            mx8 = sb("mx8", [128, 8], F32)
            Mm = sb("Mm", [128, NE], F32)
            nmx = sb("nmx", [128, 1], F32)
            ex = sb("ex", [128, NE], F32)
            den = sb("den", [128, 1], F32)
            Gt = sb("Gt", [128, NE], F32)
            kp1 = sb("kp1", [128, NE], F32)
            v01 = sb("v01", [128, NE], F32)
            key = sb("key", [128, NE], F32)
            t8 = sb("t8", [128, 8], F32)
            eq4 = sb("eq4", [128, 4, NE], F32)
            z4 = sb("z4", [128, 4], F32)
            posf = sb("posf", [128, 4], F32)
            posi = [sb(f"posi{i}", [128, 4], I32) for i in range(2)]
            sg = sb("sg", [128, D], F32)
            ee = sb("ee", [128, D], F32)
            r2t = [sb(f"r2t{i}", [128, D], F32) for i in range(2)]
            P.dma("pool", lambda e: e.dma_start(out=wout[:], in_=w_out[l].rearrange("(k p) n -> p k n", p=128)), writes=["wout"])
            P.dma("pool", lambda e: e.dma_start(out=wpg[:], in_=w_pg[l].rearrange("(k p) n -> p k n", p=128)), writes=["wpg"])
            P.dma("pool", lambda e: e.dma_start(out=wpp[:], in_=w_pp[l].rearrange("(k p) n -> p k n", p=128)), writes=["wpp"])
            P.dve(lambda e: e.memset(bpg[:], 0.0), writes=["bpg"])
            P.dve(lambda e: e.memset(br[:], 0.0), writes=["br"])
            P.dma("pool", lambda e: e.dma_start(out=bpg[0:1, :], in_=b_pg[l:l + 1, :]), writes=["bpg"])
            P.dma("sp", lambda e: e.dma_start(out=wr[:], in_=w_router[l].rearrange("(k p) n -> p k n", p=128)), writes=["wr"])
            P.dma("sp", lambda e: e.dma_start(out=br[0:1, :], in_=b_router[l:l + 1, :]), writes=["br"])
            P.dma("sp", lambda e: e.dma_start(out=l1g[:], in_=ln1g[l:l + 1, :].partition_broadcast(128)), writes=["lng"])
            P.dma("sp", lambda e: e.dma_start(out=l1b[:], in_=ln1b[l:l + 1, :].partition_broadcast(128)), writes=["lnb"])
            P.dma("sp", lambda e: e.dma_start(out=lst, in_=cin["list_init"]), writes=["lst"])
            P.dve(lambda e: e.memset(Macc[:], 0.0), writes=["Macc"])

            for t in range(NT):
                tsl = slice(t * 128, (t + 1) * 128)
                ti = t % 2
                P.dma("sp", lambda e, ti=ti, tsl=tsl: e.dma_start(out=mt[ti][:], in_=mixed[tsl, :]), writes=[f"mt{ti}"])
                P.dma("sp", lambda e, ti=ti, tsl=tsl: e.dma_start(out=xi[ti][:], in_=X_in[tsl, :]), writes=[f"xi{ti}"])
                P.dma("sp", lambda e, ti=ti, tsl=tsl: e.dma_start(out=pi[ti][:], in_=p_in[l, tsl, :]), writes=[f"pi{ti}"])
                for hb in range(2):
                    for kk in range(4):
                        k = hb * 4 + kk
                        P.pe(lambda e, hb=hb, kk=kk, k=k, ti=ti: e.matmul(ps[hb][:, kk * 128:(kk + 1) * 128], lhsT=mt[ti][:, k * 128:(k + 1) * 128],
                                                                         rhs=ident_b[:], start=True, stop=True),
                             reads=[f"mt{ti}", "ident_b"], writes=[PS(hb)])
                    if hb == 0:
                        P.act(lambda e, hb=hb: e.activation(out=mT[:, 0:4, :], in_=ps[0][:, :].rearrange("p (k n) -> p k n", k=4), func=AF.Copy),
                              reads=[PS(0)], writes=["mT"])
                    else:
                        P.dve(lambda e, hb=hb: e.tensor_copy(out=mT[:, 4:8, :], in_=ps[1][:, :].rearrange("p (k n) -> p k n", k=4)),
                              reads=[PS(1)], writes=["mT"])
                for hb in range(2):
                    for k in range(8):
                        P.pe(lambda e, hb=hb, k=k: e.matmul(ps[2 + hb][:, :], lhsT=mT[:, k, :], rhs=wout[:, k, hb * 512:(hb + 1) * 512],
                                                           start=(k == 0), stop=(k == 7)), reads=["mT", "wout"], writes=[PS(2 + hb)])
                    P.dve(lambda e, hb=hb, ti=ti: e.scalar_tensor_tensor(out=y[:, hb * 512:(hb + 1) * 512], in0=xi[ti][:, hb * 512:(hb + 1) * 512],
                                                                         scalar=ALPHA, in1=ps[2 + hb][:, :], op0=ALU.mult, op1=ALU.add),
                          reads=[f"xi{ti}", PS(2 + hb)], writes=["y"])
                layer_norm(y, x1[ti], l1g, l1b, ("y", f"x1{ti}"), st6, mv, lrs, xn)
                P.act(lambda e, ti=ti: e.activation(out=x1b[ti][:], in_=x1[ti][:], func=AF.Copy), reads=[f"x1{ti}"], writes=[f"x1b{ti}"])
                P.dma("sp", lambda e, ti=ti, tsl=tsl: e.dma_start(out=h_bf[tsl, :], in_=x1b[ti][:]), reads=[f"x1b{ti}"], writes=[f"h_bf{t}"])
                for hb in range(2):
                    for kk in range(4):
                        k = hb * 4 + kk
                        P.pe(lambda e, hb=hb, kk=kk, k=k, ti=ti: e.matmul(ps[4 + hb][:, kk * 128:(kk + 1) * 128], lhsT=x1[ti][:, k * 128:(k + 1) * 128],
                                                                         rhs=ident_f[:], start=True, stop=True),
                             reads=[f"x1{ti}", "ident"], writes=[PS(4 + hb)])
                    P.act(lambda e, hb=hb: e.activation(out=x1Tf[:, hb * 4:hb * 4 + 4, :], in_=ps[4 + hb][:, :].rearrange("p (k n) -> p k n", k=4), func=AF.Copy),
                          reads=[PS(4 + hb)], writes=["x1Tf"])
                    P.dve(lambda e, hb=hb: e.tensor_copy(out=x1Tb[:, hb * 4:hb * 4 + 4, :], in_=ps[4 + hb][:, :].rearrange("p (k n) -> p k n", k=4)),
                          reads=[PS(4 + hb)], writes=["x1Tb"])
                for k in range(8):
                    P.pe(lambda e, k=k: e.matmul(ps[6][:, 0:NE], lhsT=x1Tf[:, k, :], rhs=wr[:, k, :], start=(k == 0), stop=False),
                         reads=["x1Tf", "wr"], writes=[PS(6)])
                P.pe(lambda e: e.matmul(ps[6][:, 0:NE], lhsT=row0_f[:, :], rhs=br[:, :], start=False, stop=True),
                     reads=["row0", "br"], writes=[PS(6)])
                P.dve(lambda e: e.tensor_copy(out=lg[:], in_=ps[6][:, 0:NE]), reads=[PS(6)], writes=["lg"])
                P.dve(lambda e: e.max(out=mx8[:], in_=lg[:]), reads=["lg"], writes=["mx8"])
                P.dve(lambda e: e.tensor_scalar(out=Mm[:], in0=lg[:], scalar1=mx8[:, 3:4], scalar2=None, op0=ALU.is_ge), reads=["lg", "mx8"], writes=["Mm"])
                P.dve(lambda e: e.tensor_scalar(out=nmx[:], in0=mx8[:, 0:1], scalar1=-1.0, scalar2=None, op0=ALU.mult), reads=["mx8"], writes=["nmx"])
                P.act(lambda e: e.activation(out=ex[:], in_=lg[:], func=AF.Exp, bias=nmx[:, 0:1]), reads=["lg", "nmx"], writes=["ex"])
                P.dve(lambda e: e.tensor_tensor(out=ex[:], in0=ex[:], in1=Mm[:], op=ALU.mult), reads=["ex", "Mm"], writes=["ex"])
                P.dve(lambda e: e.tensor_reduce(out=den[:], in_=ex[:], axis=AX.X, op=ALU.add), reads=["ex"], writes=["den"])
                P.dve(lambda e: e.reciprocal(out=den[:], in_=den[:]), reads=["den"], writes=["den"])
                P.pe(lambda e: e.matmul(ps[6][:, 64:64 + NE], lhsT=lstrict[:], rhs=Mm[:], start=True, stop=False), reads=["lstrict", "Mm"], writes=[PS(6)])
                P.pe(lambda e: e.matmul(ps[6][:, 64:64 + NE], lhsT=ones_f[:], rhs=Macc[:], start=False, stop=True), reads=["ones", "Macc"], writes=[PS(6)])
                P.dve(lambda e: e.tensor_tensor(out=kp1[:], in0=ps[6][:, 64:64 + NE], in1=ecap[:], op=ALU.add), reads=[PS(6), "ecap"], writes=["kp1"])
                P.dve(lambda e: e.tensor_scalar(out=v01[:], in0=ps[6][:, 64:64 + NE], scalar1=float(C) - 0.5, scalar2=None, op0=ALU.is_lt), reads=[PS(6)], writes=["v01"])
                P.dve(lambda e: e.tensor_tensor(out=Macc[:], in0=Macc[:], in1=Mm[:], op=ALU.add), reads=["Macc", "Mm"], writes=["Macc"])
                P.dve(lambda e: e.tensor_tensor(out=v01[:], in0=v01[:], in1=Mm[:], op=ALU.mult), reads=["v01", "Mm"], writes=["v01"])
                P.dve(lambda e: e.tensor_tensor(out=key[:], in0=kp1[:], in1=v01[:], op=ALU.mult), reads=["kp1", "v01"], writes=["key"])
                P.dve(lambda e: e.scalar_tensor_tensor(out=Gt[:], in0=ex[:], scalar=den[:, 0:1], in1=v01[:], op0=ALU.mult, op1=ALU.mult),
                      reads=["ex", "den", "v01"], writes=["Gt"])
                P.dve(lambda e: e.max(out=t8[:], in_=key[:]), reads=["key"], writes=["t8"])
                for j in range(4):
                    P.dve(lambda e, j=j: e.tensor_scalar(out=eq4[:, j, :], in0=key[:], scalar1=t8[:, j:j + 1], scalar2=None, op0=ALU.is_equal),
                          reads=["key", "t8"], writes=["eq4"])
                P.dve(lambda e: e.tensor_tensor(out=eq4[:], in0=eq4[:], in1=Gt[:, :].unsqueeze(1).to_broadcast([128, 4, NE]), op=ALU.mult),
                      reads=["eq4", "Gt"], writes=["eq4"])
                P.dve(lambda e, t=t: e.tensor_reduce(out=g4all[:, t, :], in_=eq4[:], axis=AX.X, op=ALU.add), reads=["eq4"], writes=["g4all"])
                P.dve(lambda e: e.tensor_scalar(out=z4[:], in0=t8[:, 0:4], scalar1=0.5, scalar2=BIGPOS, op0=ALU.is_lt, op1=ALU.mult), reads=["t8"], writes=["z4"])
                P.dve(lambda e: e.scalar_tensor_tensor(out=posf[:], in0=t8[:, 0:4], scalar=-1.0, in1=z4[:], op0=ALU.add, op1=ALU.add),
                      reads=["t8", "z4"], writes=["posf"])
                P.dve(lambda e, ti=ti: e.tensor_copy(out=posi[ti][:], in_=posf[:]), reads=["posf"], writes=[f"posi{ti}"])
                for j in range(4):
                    P.dma("pool", lambda e, j=j, t=t, ti=ti: e.indirect_dma_start(
                        out=lst, out_offset=bass.IndirectOffsetOnAxis(ap=posi[ti][:, j:j + 1], axis=0),
                        in_=tokslot[:, (t * 4 + j) * 2:(t * 4 + j) * 2 + 2], in_offset=None,
                        bounds_check=P.reg(e, NE * C - 1), oob_is_err=False), reads=[f"posi{ti}", "tokslot", "lst"], writes=[f"lst_w{t}_{j}"])
                for kk in range(2):
                    P.pe(lambda e, kk=kk, ti=ti: e.matmul(ps[7][:, kk * 128:(kk + 1) * 128], lhsT=pi[ti][:, kk * 128:(kk + 1) * 128], rhs=ident_f[:],
                                                         start=True, stop=True), reads=[f"pi{ti}", "ident"], writes=[PS(7)])
                P.act(lambda e: e.activation(out=pTt[:], in_=ps[7][:, 0:256].rearrange("p (k n) -> p k n", k=2), func=AF.Copy), reads=[PS(7)], writes=["pTt"])
                for hb in range(2):
                    for k in range(8):
                        P.pe(lambda e, hb=hb, k=k: e.matmul(ps[hb][:, :], lhsT=x1Tb[:, k, :], rhs=wpg[:, k, hb * 512:(hb + 1) * 512],
                                                           start=(k == 0), stop=False), reads=["x1Tb", "wpg"], writes=[PS(hb)])
                    P.pe(lambda e, hb=hb: e.matmul(ps[hb][:, :], lhsT=row0_b[:, :], rhs=bpg[:, hb * 512:(hb + 1) * 512], start=False, stop=True),
                         reads=["row0_b", "bpg"], writes=[PS(hb)])
                    P.act(lambda e, hb=hb: e.activation(out=sg[:, hb * 512:(hb + 1) * 512], in_=ps[hb][:, :], func=AF.Sigmoid), reads=[PS(hb)], writes=["sg"])
                    for kk in range(2):
                        P.pe(lambda e, hb=hb, kk=kk: e.matmul(ps[2 + hb][:, :], lhsT=pTt[:, kk, :], rhs=wpp[:, kk, hb * 512:(hb + 1) * 512],
                                                             start=(kk == 0), stop=(kk == 1)), reads=["pTt", "wpp"], writes=[PS(2 + hb)])
                    P.dve(lambda e, hb=hb: e.tensor_tensor(out=ee[:, hb * 512:(hb + 1) * 512], in0=sg[:, hb * 512:(hb + 1) * 512], in1=ps[2 + hb][:, :], op=ALU.mult),
                          reads=["sg", PS(2 + hb)], writes=["ee"])
                P.dve(lambda e, ti=ti: e.scalar_tensor_tensor(out=r2t[ti][:], in0=x1[ti][:], scalar=ALPHA, in1=ee[:], op0=ALU.mult, op1=ALU.add),
                       reads=[f"x1{ti}", "ee"], writes=[f"r2t{ti}"])
                P.dma("sp", lambda e, ti=ti, tsl=tsl: e.dma_start(out=r2[tsl, :], in_=r2t[ti][:]), reads=[f"r2t{ti}"], writes=[f"r2_{t}"])
        _phase5()
        if stop == ("c", l):
            return True

        def _phase6():
            phase_begin()
            wgu = [sb(f"wgu{i}", [128, 8, 2 * D], BF16) for i in range(2)]
            wdn = [sb(f"wdn{i}", [128, 8, D], BF16) for i in range(2)]
            bgu = sb("bgu", [128, NE, 16], F32)
            bdn = [sb(f"bdn{i}", [128, D], BF16) for i in range(2)]
            ltl = [sb(f"ltl{i}", [128, NR, 2], I32) for i in range(2)]
            hg = [sb(f"hg{i}", [128, D], BF16) for i in range(2)]
            hT = sb("hT", [128, 8, C], BF16)
            actT = sb("actT", [128, 8, C], BF16)
            g1 = [sb(f"g1{i}", [128, 512], F32) for i in range(2)]
            sgm = [sb(f"sgm{i}", [128, 512], F32) for i in range(2)]
            u1 = [sb(f"u1{i}", [128, 512], F32) for i in range(2)]
            yo = [sb(f"yo{i}", [128, D], F32) for i in range(2)]
            P.dma("sp", lambda e: e.dma_start(out=bgu[:], in_=b_gu[l]), writes=["bgu"])
            for i in range(2):
                P.dve(lambda e, i=i: e.memset(bdn[i][:], 0.0), writes=[f"bdn{i}"])
            segs = []
            lo = 0
            while lo < C:
                n = min(384 if C % 384 == 0 else 512, C - lo)
                segs.append((lo, n))
                lo += n
            ci = 0
            for ex_ in range(NE):
                ei = ex_ % 2
                for q in range(4):
                    P.dma("pool", lambda e, q=q, ei=ei, ex_=ex_: e.dma_start(
                        out=wgu[ei][:, 2 * q:2 * q + 2, :], in_=w_gu[l, ex_, q * 256:(q + 1) * 256, :].rearrange("(k p) n -> p k n", p=128)),
                        writes=[f"wgu{ei}_{q}"])
                for q in range(2):
                    P.dma("pool", lambda e, q=q, ei=ei, ex_=ex_: e.dma_start(
                        out=wdn[ei][:, 4 * q:4 * q + 4, :], in_=w_down[l, ex_, q * 512:(q + 1) * 512, :].rearrange("(k p) n -> p k n", p=128)),
                        writes=[f"wdn{ei}_{q}"])
                P.dma("pool", lambda e, ei=ei, ex_=ex_: e.dma_start(out=bdn[ei][0:1, :], in_=b_down[l, ex_:ex_ + 1, :]), writes=[f"bdn{ei}"])
                P.dma("sp", lambda e, ei=ei, ex_=ex_: e.dma_start(out=ltl[ei][:], in_=lst[ex_ * C:(ex_ + 1) * C, :].rearrange("(r p) c -> p r c", p=128)),
                      reads=["lst"], writes=[f"ltl{ei}"])
                WGU = [f"wgu{ei}_{q}" for q in range(4)]
                WDN = [f"wdn{ei}_{q}" for q in range(2)]
                for r in range(NR):
                    gi = r % 2
                    P.dma("pool", lambda e, gi=gi, ei=ei, r=r: e.indirect_dma_start(
                        out=hg[gi][:], out_offset=None, in_=h_bf, in_offset=bass.IndirectOffsetOnAxis(ap=ltl[ei][:, r, 0:1], axis=0),
                        bounds_check=P.reg(e, S), oob_is_err=False), reads=[f"ltl{ei}", "h_bf"], writes=[f"hg{gi}"])
                    for hb in range(2):
                        for kk in range(4):
                            k = hb * 4 + kk
                            P.pe(lambda e, hb=hb, kk=kk, k=k, gi=gi: e.matmul(ps[hb][:, kk * 128:(kk + 1) * 128], lhsT=hg[gi][:, k * 128:(k + 1) * 128],
                                                                             rhs=ident_b[:], start=True, stop=True),
                                 reads=[f"hg{gi}", "ident_b"], writes=[PS(hb)])
                        if hb == 0:
                            P.act(lambda e, r=r: e.activation(out=hT[:, 0:4, r * 128:(r + 1) * 128], in_=ps[0][:, :].rearrange("p (k n) -> p k n", k=4), func=AF.Copy),
                                  reads=[PS(0)], writes=["hT"])
                        else:
                            P.dve(lambda e, r=r: e.tensor_copy(out=hT[:, 4:8, r * 128:(r + 1) * 128], in_=ps[1][:, :].rearrange("p (k n) -> p k n", k=4)),
                                  reads=[PS(1)], writes=["hT"])
                pend = [None]
                for jf in range(8):
                    for (lo, n) in segs:
                        a = ci % 2
                        ci += 1
                        bg_, bu_ = 2 + 2 * a, 3 + 2 * a
                        for k in range(8):
                            P.pe(lambda e, k=k, jf=jf, lo=lo, n=n, bg_=bg_, ei=ei: e.matmul(ps[bg_][:, 0:n], lhsT=wgu[ei][:, k, jf * 128:(jf + 1) * 128],
                                                                                       rhs=hT[:, k, lo:lo + n], start=(k == 0), stop=(k == 7)),
                                 reads=["hT", f"wgu{ei}_{k // 2}"], writes=[PS(bg_)])
                        for k in range(8):
                            P.pe(lambda e, k=k, jf=jf, lo=lo, n=n, bu_=bu_, ei=ei: e.matmul(ps[bu_][:, 0:n], lhsT=wgu[ei][:, k, D + jf * 128:D + (jf + 1) * 128],
                                                                                       rhs=hT[:, k, lo:lo + n], start=(k == 0), stop=(k == 7)),
                                 reads=["hT", f"wgu{ei}_{k // 2}"], writes=[PS(bu_)])
                        P.dve(lambda e, a=a, bg_=bg_, n=n, ex_=ex_, jf=jf: e.tensor_scalar(out=g1[a][:, 0:n], in0=ps[bg_][:, 0:n], scalar1=bgu[:, ex_, jf:jf + 1],
                                                                                        scalar2=7.0, op0=ALU.add, op1=ALU.min),
                              reads=[PS(bg_), "bgu"], writes=[f"g1{a}"])
                        P.act(lambda e, a=a, n=n: e.activation(out=sgm[a][:, 0:n], in_=g1[a][:, 0:n], func=AF.Sigmoid, scale=1.702),
                              reads=[f"g1{a}"], writes=[f"sgm{a}"])
                        P.dve(lambda e, a=a, bu_=bu_, n=n, ex_=ex_, jf=jf: e.tensor_scalar(out=u1[a][:, 0:n], in0=ps[bu_][:, 0:n], scalar1=bgu[:, ex_, 8 + jf:9 + jf],
                                                                                        scalar2=7.0, op0=ALU.add, op1=ALU.min),
                              reads=[PS(bu_), "bgu"], writes=[f"u1{a}"])
                        P.dve(lambda e, a=a, n=n: e.tensor_scalar(out=u1[a][:, 0:n], in0=u1[a][:, 0:n], scalar1=-7.0, scalar2=1.0, op0=ALU.max, op1=ALU.add),
                               reads=[f"u1{a}"], writes=[f"u1{a}"])
                        P.pool(lambda e, a=a, n=n: e.tensor_tensor(out=g1[a][:, 0:n], in0=g1[a][:, 0:n], in1=sgm[a][:, 0:n], op=ALU.mult),
                               reads=[f"g1{a}", f"sgm{a}"], writes=[f"g1{a}"])
                        if pend[0] is not None:
                            pend[0]()
                        pend[0] = _freeze(lambda a=a, n=n, jf=jf, lo=lo: P.dve(
                            lambda e, a=a, n=n, jf=jf, lo=lo: e.tensor_tensor(out=actT[:, jf, lo:lo + n], in0=g1[a][:, 0:n], in1=u1[a][:, 0:n], op=ALU.mult),
                            reads=[f"g1{a}", f"u1{a}"], writes=["actT"]))
                pend[0]()
                pend[0] = None
                for r in range(NR):
                    yi = r % 2
                    for hb in range(2):
                        bank = 6 + hb
                        for k in range(8):
                            P.pe(lambda e, k=k, r=r, hb=hb, bank=bank, ei=ei: e.matmul(ps[bank][:, :], lhsT=actT[:, k, r * 128:(r + 1) * 128],
                                                                                     rhs=wdn[ei][:, k, hb * 512:(hb + 1) * 512], start=(k == 0), stop=False),
                                 reads=["actT", f"wdn{ei}_{k // 4}"], writes=[PS(bank)])
                        P.pe(lambda e, hb=hb, bank=bank, ei=ei: e.matmul(ps[bank][:, :], lhsT=row0_b[:, :], rhs=bdn[ei][:, hb * 512:(hb + 1) * 512],
                                                                        start=False, stop=True), reads=["row0_b", f"bdn{ei}"], writes=[PS(bank)])
                        if hb == 0:
                            P.act(lambda e, yi=yi, bank=bank: e.activation(out=yo[yi][:, 0:512], in_=ps[bank][:, :], func=AF.Copy), reads=[PS(bank)], writes=[f"yo{yi}"])
                        else:
                            P.dve(lambda e, yi=yi, bank=bank: e.tensor_copy(out=yo[yi][:, 512:1024], in_=ps[bank][:, :]), reads=[PS(bank)], writes=[f"yo{yi}"])
                    P.dma("pool", lambda e, yi=yi, ei=ei, r=r: e.indirect_dma_start(
                        out=ybuf, out_offset=bass.IndirectOffsetOnAxis(ap=ltl[ei][:, r, 1:2], axis=0), in_=yo[yi][:], in_offset=None,
                        bounds_check=P.reg(e, 4 * S - 1), oob_is_err=False), reads=[f"yo{yi}", f"ltl{ei}"], writes=[f"ybuf_{ex_}_{r}"])
        _phase6()
        if stop == ("d", l):
            return True

        def _phase7():
            phase_begin()
            l2g = sb("l2g", [128, D], F32)
            l2b = sb("l2b", [128, D], F32)
            rr = [sb(f"rr{i}", [128, D], F32) for i in range(2)]
            yb = [sb(f"yb{i}", [128, 4, D], F32) for i in range(2)]
            xo = [sb(f"xo{i}", [128, D], F32) for i in range(2)]
            st6 = sb("st6e", [128, 12], F32)
            mv = sb("mve", [128, 2], F32)
            lrs = sb("lrse", [128, 1], F32)
            xn = sb("xne", [128, D], F32)
            P.dma("sp", lambda e: e.dma_start(out=l2g[:], in_=ln2g[l:l + 1, :].partition_broadcast(128)), writes=["lng"])
            P.dma("sp", lambda e: e.dma_start(out=l2b[:], in_=ln2b[l:l + 1, :].partition_broadcast(128)), writes=["lnb"])
            ybv2 = ybuf.rearrange("(t p j) d -> t p j d", p=128, j=4)
            for t in range(NT):
                tsl = slice(t * 128, (t + 1) * 128)
                ti = t % 2
                P.dma("sp", lambda e, ti=ti, tsl=tsl: e.dma_start(out=rr[ti][:], in_=r2[tsl, :]), writes=[f"rr{ti}"])
                P.dma("pool", lambda e, ti=ti, t=t: e.dma_start(out=yb[ti][:], in_=ybv2[t]), writes=[f"yb{ti}"])
                for j in range(4):
                    eng = P.dve
                    eng(lambda e, ti=ti, j=j, t=t: e.scalar_tensor_tensor(out=rr[ti][:], in0=yb[ti][:, j, :], scalar=g4all[:, t, j:j + 1], in1=rr[ti][:],
                                                                          op0=ALU.mult, op1=ALU.add), reads=[f"yb{ti}", f"rr{ti}", "g4all"], writes=[f"rr{ti}"])
                layer_norm(rr[ti], xo[ti], l2g, l2b, (f"rr{ti}", f"xo{ti}"), st6, mv, lrs, xn)
                P.dma("sp", lambda e, ti=ti, tsl=tsl: e.dma_start(out=X_out[tsl, :], in_=xo[ti][:]), reads=[f"xo{ti}"], writes=[f"xout{t}"])
        _phase7()
        return False

    for l_ in range(L):
        if layer(l_):
            break
    P.emit(max_ops)
    return nc, P


def prep_shared(inp, S, C, L):
    perm = win_col_perm()
    sh = {}
    sh["w_in"] = np.ascontiguousarray(inp["w_in"][:L][:, :, perm])
    sh["wg_aug"] = np.ascontiguousarray(np.concatenate([inp["w_gla_gate"][:L], inp["b_gla_gate"][:L, None, :]], axis=1))
    sh["gla_norm_g"] = np.ascontiguousarray(inp["gla_norm_g"][:L])
    sh["diff_lambda"] = np.ascontiguousarray(inp["diff_lambda"][:L])
    sh["diff_norm_g"] = np.ascontiguousarray(inp["diff_norm_g"][:L])
    sh["w_out"] = np.ascontiguousarray(inp["w_out"][:L])
    sh["rb_aug"] = np.ascontiguousarray(np.concatenate([inp["rel_bias"], np.full((1, 4), NEG, np.float32)], axis=0))
    for k in ("ln1_g", "ln1_b", "w_router", "b_router", "w_down", "b_down", "ln2_g", "ln2_b"):
        sh[k] = np.ascontiguousarray(inp[k][:L])
    gu_perm = np.concatenate([np.arange(0, 2 * D, 2), np.arange(1, 2 * D, 2)])
    sh["w_gu"] = np.ascontiguousarray(inp["w_gate_up"][:L][:, :, :, gu_perm])
    bg = inp["b_gate_up"][:L][:, :, gu_perm]
    sh["b_gu"] = np.ascontiguousarray(bg.reshape(L, NE, 16, 128).transpose(0, 3, 1, 2))
    sh["w_pg"] = np.ascontiguousarray(inp["w_ple_gate"][:L])
    sh["b_pg"] = np.ascontiguousarray(inp["b_ple_gate"][:L])
    sh["w_pp"] = np.ascontiguousarray(inp["w_ple_proj"][:L])
    for k, v in host_consts(S, C).items():
        sh["c_" + k] = v
    return sh


def kernel(**inputs):
    inp = {k: np.asarray(v) for k, v in inputs.items()}
    B, S, _ = inp["x"].shape
    L = inp["w_in"].shape[0]
    C = 768
    nc, _ = build(S, L, C)
    sh = prep_shared(inp, S, C, L)
    in_maps = []
    for b in range(B):
        m = dict(sh)
        m["x"] = np.ascontiguousarray(inp["x"][b])
        m["p"] = np.ascontiguousarray(inp["p"][:, b])
        in_maps.append(m)
    res = run_bass_kernel_spmd(nc, in_maps, core_ids=list(range(B)))
    return np.stack([np.asarray(r["out"]) for r in res.results], axis=0).astype(np.float32)
```
